# Optimizing a Trainium2 kernel written in Bass

```python
import math
import jax, jax.numpy as jnp
from jax import lax
import numpy as np

D_MODEL = 1024
BATCH = 32
SEQ = 2048
DEPTH = 4

NORM_EPS = 1e-6
D_FF = 256 * ((8 * D_MODEL // 3 + 255) // 256)
CHUNK = 64
MIX_W = D_MODEL
M_DH = 64
M_W = MIX_W // 4
M_HEADS = M_W // M_DH
R_DH = 64
R_W = MIX_W // 4
R_HEADS = R_W // R_DH
ROPE_BASE = 10000.0
G_DH = 128
G_W = MIX_W - M_W - R_W
G_HEADS = G_W // G_DH
CONV_K = 4
IN_SIZES = (M_W, M_W, M_W, M_W, M_HEADS, M_HEADS,
            R_W, R_W, R_W, R_W,
            G_W, G_W, G_W, G_W, G_HEADS, G_HEADS)
IN_W = sum(IN_SIZES)

kernel_name = 'hybrid_mlstm_retnet_gdn_macaron'


def _rmsnorm(x, gain):
    x32 = x.astype(jnp.float32)
    y = x32 * lax.rsqrt(jnp.mean(x32 * x32, axis=-1, keepdims=True) + NORM_EPS)
    return (y * gain).astype(x.dtype)


def _swiglu(x, w_gate, w_up, w_down):
    hid = jax.nn.silu(jnp.einsum('bsd,df->bsf', x, w_gate)) * jnp.einsum('bsd,df->bsf', x, w_up)
    return jnp.einsum('bsf,fd->bsd', hid, w_down)


def _heads(t, n_heads):
    b, s, w = t.shape
    return t.reshape(b, s, n_heads, w // n_heads).transpose(0, 2, 1, 3)


def _head_rmsnorm(h, gain):
    h = h * lax.rsqrt(jnp.mean(h * h, axis=-1, keepdims=True) + NORM_EPS)
    b, nh, s, d = h.shape
    return h.transpose(0, 2, 1, 3).reshape(b, s, nh * d) * gain


def _l2norm(t):
    return t * lax.rsqrt(jnp.sum(t * t, axis=-1, keepdims=True) + NORM_EPS)


def _rope(t):
    d = t.shape[-1]
    inv_freq = ROPE_BASE ** (-jnp.arange(0, d, 2, dtype=jnp.float32) / d)
    ang = jnp.arange(t.shape[2], dtype=jnp.float32)[:, None] * inv_freq[None, :]
    cos, sin = jnp.cos(ang), jnp.sin(ang)
    t1, t2 = t[..., : d // 2], t[..., d // 2:]
    return jnp.concatenate([t1 * cos - t2 * sin, t1 * sin + t2 * cos], axis=-1)


def _causal_dwconv(x, w):
    k, c = w.shape
    return lax.conv_general_dilated(x, w[:, None, :].astype(x.dtype), window_strides=(1,),
                                    padding=[(k - 1, 0)], dimension_numbers=('NWC', 'WIO', 'NWC'),
                                    feature_group_count=c)


def _to_chunks(t):
    b, h, s = t.shape[:3]
    return jnp.moveaxis(t.reshape(b, h, s // CHUNK, CHUNK, *t.shape[3:]), 2, 0)


def _from_chunks(t):
    n, b, h, l, d = t.shape
    return jnp.moveaxis(t, 0, 2).reshape(b, h, n * l, d)


def _mlstm(q, k, v, i_pre, f_pre):
    b, nh, s, d = q.shape
    k = k * d ** -0.5
    log_f = jax.nn.log_sigmoid(f_pre)
    causal = jnp.tril(jnp.ones((CHUNK, CHUNK), dtype=bool))

    def step(carry, xs):
        c_st, n_st, m_st = carry
        qc, kc, vc, ic, fc = xs
        bcum = jnp.cumsum(fc, axis=-1)
        log_w = jnp.where(causal, bcum[..., :, None] - bcum[..., None, :] + ic[..., None, :], -jnp.inf)
        log_inter = bcum + m_st[..., None]
        m_row = jnp.maximum(log_inter, jnp.max(log_w, axis=-1))
        w = jnp.exp(log_w - m_row[..., None])
        inter = jnp.exp(log_inter - m_row)
        sc = jnp.einsum('bhid,bhjd->bhij', qc, kc) * w
        num = jnp.einsum('bhij,bhje->bhie', sc, vc) + inter[..., None] * jnp.einsum('bhid,bhde->bhie', qc, c_st)
        den = jnp.sum(sc, axis=-1) + inter * jnp.einsum('bhid,bhd->bhi', qc, n_st)
        hc = num / jnp.maximum(jnp.abs(den), jnp.exp(-m_row))[..., None]
        b_last = bcum[..., -1]
        log_src = b_last[..., None] - bcum + ic
        m_new = jnp.maximum(b_last + m_st, jnp.max(log_src, axis=-1))
        carry_decay = jnp.exp(b_last + m_st - m_new)
        kw = kc * jnp.exp(log_src - m_new[..., None])[..., None]
        c_st = carry_decay[..., None, None] * c_st + jnp.einsum('bhjd,bhje->bhde', kw, vc)
        n_st = carry_decay[..., None] * n_st + jnp.sum(kw, axis=2)
        return (c_st, n_st, m_new), hc

    init = (jnp.zeros((b, nh, d, v.shape[-1]), q.dtype), jnp.zeros((b, nh, d), q.dtype),
            jnp.zeros((b, nh), q.dtype))
    _, hs = lax.scan(step, init, (_to_chunks(q), _to_chunks(k), _to_chunks(v),
                                  _to_chunks(i_pre), _to_chunks(log_f)))
    return _from_chunks(hs)


def _retention(q, k, v):
    b, nh, s, d = q.shape
    k = k * d ** -0.5
    log_gamma = jnp.log1p(-jnp.exp2(-5.0 - jnp.arange(nh, dtype=jnp.float32)))
    pos = jnp.arange(CHUNK, dtype=jnp.float32)
    rel = pos[:, None] - pos[None, :]
    causal = rel >= 0
    intra = jnp.where(causal, jnp.exp(jnp.where(causal, rel, 0.0) * log_gamma[:, None, None]), 0.0)
    q_decay = jnp.exp((pos + 1.0) * log_gamma[:, None])[..., None]
    k_decay = jnp.exp((CHUNK - 1.0 - pos) * log_gamma[:, None])[..., None]
    chunk_decay = jnp.exp(CHUNK * log_gamma)[:, None, None]

    def step(r_st, xs):
        qc, kc, vc = xs
        sc = jnp.einsum('bhid,bhjd->bhij', qc, kc) * intra
        o = jnp.einsum('bhij,bhje->bhie', sc, vc) + jnp.einsum('bhid,bhde->bhie', qc * q_decay, r_st)
        r_st = chunk_decay * r_st + jnp.einsum('bhjd,bhje->bhde', kc * k_decay, vc)
        return r_st, o

    init = jnp.zeros((b, nh, d, v.shape[-1]), q.dtype)
    _, outs = lax.scan(step, init, (_to_chunks(q), _to_chunks(k), _to_chunks(v)))
    return _from_chunks(outs)


def _gated_delta(q, k, v, log_alpha, beta):
    b, nh, s, dk = q.shape
    dv = v.shape[-1]
    q = q * dk ** -0.5
    idx = jnp.arange(CHUNK)
    causal = idx[:, None] >= idx[None, :]
    strict = idx[:, None] > idx[None, :]
    eye = jnp.eye(CHUNK, dtype=q.dtype)

    def step(s_st, xs):
        qc, kc, vc, ac, bc = xs
        g = jnp.cumsum(ac, axis=-1)
        diff = g[..., :, None] - g[..., None, :]
        decay = jnp.where(causal, jnp.exp(jnp.where(causal, diff, 0.0)), 0.0)
        kb = kc * bc[..., None]
        a_mat = eye + jnp.where(strict, jnp.einsum('bhid,bhjd->bhij', kb, kc) * decay, 0.0)
        rhs = jnp.concatenate([vc * bc[..., None], kb * jnp.exp(g)[..., None]], axis=-1)
        sol = lax.linalg.triangular_solve(a_mat, rhs, left_side=True, lower=True, unit_diagonal=True)
        u = sol[..., :dv] - jnp.einsum('bhik,bhkv->bhiv', sol[..., dv:], s_st)
        qk = jnp.einsum('bhik,bhjk->bhij', qc, kc) * decay
        o = jnp.einsum('bhik,bhkv->bhiv', qc * jnp.exp(g)[..., None], s_st) + jnp.einsum('bhij,bhjv->bhiv', qk, u)
        g_last = g[..., -1:]
        s_st = jnp.exp(g_last)[..., None] * s_st + jnp.einsum('bhjk,bhjv->bhkv', kc * jnp.exp(g_last - g)[..., None], u)
        return s_st, o

    init = jnp.zeros((b, nh, dk, dv), q.dtype)
    _, outs = lax.scan(step, init, (_to_chunks(q), _to_chunks(k), _to_chunks(v),
                                    _to_chunks(log_alpha), _to_chunks(beta)))
    return _from_chunks(outs)


def _hybrid_mixer(u, w_in, m_i_bias, m_f_bias, g_conv, g_a_log, g_dt_bias,
                  m_out_norm, r_out_norm, g_out_norm, w_out):
    f32 = jnp.float32
    offsets = [int(o) for o in np.cumsum(IN_SIZES)[:-1]]
    proj = jnp.einsum('bsd,dp->bsp', u, w_in).astype(f32)
    (mq, mk, mv, mo, mi, mf, rq, rk, rv, rg, gq, gk, gv, gz, ga, gb) = jnp.split(proj, offsets, axis=-1)
    i_pre = jnp.transpose(mi + m_i_bias, (0, 2, 1))
    f_pre = jnp.transpose(mf + m_f_bias, (0, 2, 1))
    h_m = _mlstm(_heads(mq, M_HEADS), _heads(mk, M_HEADS), _heads(mv, M_HEADS), i_pre, f_pre)
    y_m = _head_rmsnorm(h_m, m_out_norm) * jax.nn.sigmoid(mo)
    h_r = _retention(_rope(_heads(rq, R_HEADS)), _rope(_heads(rk, R_HEADS)), _heads(rv, R_HEADS))
    y_r = _head_rmsnorm(h_r, r_out_norm) * jax.nn.silu(rg)
    qkv = jax.nn.silu(_causal_dwconv(jnp.concatenate([gq, gk, gv], axis=-1), g_conv))
    cq, ck, cv = jnp.split(qkv, 3, axis=-1)
    log_alpha = -jnp.exp(g_a_log) * jax.nn.softplus(ga + g_dt_bias)
    beta = jax.nn.sigmoid(gb)
    h_g = _gated_delta(_l2norm(_heads(cq, G_HEADS)), _l2norm(_heads(ck, G_HEADS)), _heads(cv, G_HEADS),
                       jnp.transpose(log_alpha, (0, 2, 1)), jnp.transpose(beta, (0, 2, 1)))
    y_g = _head_rmsnorm(h_g, g_out_norm) * jax.nn.silu(gz)
    y = jnp.concatenate([y_m, y_r, y_g], axis=-1).astype(u.dtype)
    return jnp.einsum('bsm,md->bsd', y, w_out)


def setup_inputs(seed: int = 0) -> dict:
    key = jax.random.key(seed)
    ks = jax.random.split(key, 24)
    f32 = jnp.float32

    def nrm(k, shape, fan_in):
        return jax.random.normal(k, shape, f32) * fan_in ** -0.5

    def gain(k, shape):
        return 1.0 + 0.02 * jax.random.normal(k, shape, f32)

    x = jax.random.normal(ks[0], (BATCH, SEQ, D_MODEL), f32)
    ffn1_norm = gain(ks[1], (DEPTH, D_MODEL))
    ffn1_w_gate = nrm(ks[2], (DEPTH, D_MODEL, D_FF), D_MODEL)
    ffn1_w_up = nrm(ks[3], (DEPTH, D_MODEL, D_FF), D_MODEL)
    ffn1_w_down = nrm(ks[4], (DEPTH, D_FF, D_MODEL), D_FF)
    mix_norm = gain(ks[5], (DEPTH, D_MODEL))
    w_in = nrm(ks[6], (DEPTH, D_MODEL, IN_W), D_MODEL)
    m_i_bias = 0.1 * jax.random.normal(ks[7], (DEPTH, M_HEADS), f32)
    m_f_bias = jnp.linspace(3.0, 6.0, M_HEADS, dtype=f32)[None, :] + 0.1 * jax.random.normal(ks[8], (DEPTH, M_HEADS), f32)
    g_conv = nrm(ks[9], (DEPTH, CONV_K, 3 * G_W), CONV_K)
    g_a_log = jnp.log(jax.random.uniform(ks[10], (DEPTH, G_HEADS), f32, 1.0, 16.0))
    dt = jnp.exp(jax.random.uniform(ks[11], (DEPTH, G_HEADS), f32, math.log(1e-3), math.log(1e-1)))
    g_dt_bias = dt + jnp.log(-jnp.expm1(-dt))
    m_out_norm = gain(ks[12], (DEPTH, M_W))
    r_out_norm = gain(ks[13], (DEPTH, R_W))
    g_out_norm = gain(ks[14], (DEPTH, G_W))
    w_out = nrm(ks[15], (DEPTH, MIX_W, D_MODEL), MIX_W)
    ffn2_norm = gain(ks[16], (DEPTH, D_MODEL))
    ffn2_w_gate = nrm(ks[17], (DEPTH, D_MODEL, D_FF), D_MODEL)
    ffn2_w_up = nrm(ks[18], (DEPTH, D_MODEL, D_FF), D_MODEL)
    ffn2_w_down = nrm(ks[19], (DEPTH, D_FF, D_MODEL), D_FF)
    final_norm = gain(ks[20], (D_MODEL,))
    return {'x': x, 'ffn1_norm': ffn1_norm, 'ffn1_w_gate': ffn1_w_gate, 'ffn1_w_up': ffn1_w_up,
            'ffn1_w_down': ffn1_w_down, 'mix_norm': mix_norm, 'w_in': w_in, 'm_i_bias': m_i_bias,
            'm_f_bias': m_f_bias, 'g_conv': g_conv, 'g_a_log': g_a_log, 'g_dt_bias': g_dt_bias,
            'm_out_norm': m_out_norm, 'r_out_norm': r_out_norm, 'g_out_norm': g_out_norm, 'w_out': w_out,
            'ffn2_norm': ffn2_norm, 'ffn2_w_gate': ffn2_w_gate, 'ffn2_w_up': ffn2_w_up,
            'ffn2_w_down': ffn2_w_down, 'final_norm': final_norm}


def reference(x, ffn1_norm, ffn1_w_gate, ffn1_w_up, ffn1_w_down, mix_norm, w_in, m_i_bias, m_f_bias,
              g_conv, g_a_log, g_dt_bias, m_out_norm, r_out_norm, g_out_norm, w_out,
              ffn2_norm, ffn2_w_gate, ffn2_w_up, ffn2_w_down, final_norm):
    h = x
    for l in range(DEPTH):
        h = h + 0.5 * _swiglu(_rmsnorm(h, ffn1_norm[l]), ffn1_w_gate[l], ffn1_w_up[l], ffn1_w_down[l])
        h = h + _hybrid_mixer(_rmsnorm(h, mix_norm[l]), w_in[l], m_i_bias[l], m_f_bias[l], g_conv[l],
                              g_a_log[l], g_dt_bias[l], m_out_norm[l], r_out_norm[l], g_out_norm[l], w_out[l])
        h = h + 0.5 * _swiglu(_rmsnorm(h, ffn2_norm[l]), ffn2_w_gate[l], ffn2_w_up[l], ffn2_w_down[l])
    return _rmsnorm(h, final_norm)
```

```python
import itertools
import math
import os
from contextlib import ExitStack

import numpy as np
import concourse.bass as bass
import concourse.mybir as mybir
from concourse.bass_utils import run_bass_kernel_spmd

F32 = mybir.dt.float32
BF16 = mybir.dt.bfloat16
AF = mybir.ActivationFunctionType
ALU = mybir.AluOpType
AX = mybir.AxisListType

D = 1024
S = 2048
DFF = 2816
NL = 4
NFC = DFF // 128
NDC = D // 128
NCH = S // 128
INW = 4112
EPS = 1e-6
SEM_BLOCK = 16000
NEG = -30000.0
LPW = 16 + 1024 + 48
ARENA_BYTES = 104 * 1024

C_ID, C_TRIU, C_MGT, C_NEGT, C_NEGS, C_ONES, C_RW, C_RQS, C_RKS, C_RDEC, C_EPS = \
    0, 128, 256, 384, 512, 640, 768, 1280, 1284, 1288, 1292
CW = 1296

ENG = ('pe', 'act', 'dve', 'pool', 'sp')


def K(name, *ranges):
    rs = [r if isinstance(r, (list, tuple, range)) else [r] for r in ranges]
    return [(name,) + t for t in itertools.product(*rs)]


class Slot:
    def __init__(self, name):
        self.name = name
        self.count = 0
        self.sem = None


class Op:
    __slots__ = ('eng', 'fn', 'idx', 'waits', 'signal', 'count', 'slot', 'slot_count', 'snap', 'dsnap')

    def __init__(self, eng, fn, idx):
        self.eng = eng
        self.fn = fn
        self.idx = idx
        self.waits = []
        self.signal = False
        self.count = None
        self.slot = None
        self.slot_count = 0
        self.snap = None
        self.dsnap = None


class Prog:
    def __init__(self):
        self.ops = {e: [] for e in ENG}
        self.last_w = {}
        self.last_r = {}
        self.seen = {e: {f: -1 for f in ENG} for e in ENG}
        self.dseen = {e: {} for e in ENG}
        self.slots = []

    def slot(self, name):
        s = Slot(name)
        self.slots.append(s)
        return s

    def _wait_on(self, op, eng, d):
        seen = self.seen[eng]
        dseen = self.dseen[eng]
        if d.slot is not None:
            if dseen.get(d.slot, 0) < d.slot_count:
                dseen[d.slot] = d.slot_count
                op.waits.append(('dma', d.slot, d.slot_count))
                for f, v in d.snap.items():
                    if v > seen[f]:
                        seen[f] = v
        else:
            if d.idx > seen[d.eng]:
                d.signal = True
                op.waits.append(('op', d))
                seen[d.eng] = d.idx
                for g, v in d.snap.items():
                    if v > seen[g]:
                        seen[g] = v
                for s_, v in d.dsnap.items():
                    if dseen.get(s_, 0) < v:
                        dseen[s_] = v

    def emit(self, eng, fn, reads=(), writes=(), slot=None):
        op = Op(eng, fn, len(self.ops[eng]))
        is_dma = slot is not None
        pbr = [k for k in reads if k[0] == 'pb']
        if pbr:
            writes = list(writes) + [k for k in pbr if k not in writes]
        deps = []
        for k in reads:
            w = self.last_w.get(k)
            if w is not None:
                deps.append(w)
        for k in writes:
            w = self.last_w.get(k)
            if w is not None and (w.eng != eng or w.slot is not None or is_dma):
                deps.append(w)
            rd = self.last_r.get(k)
            if rd:
                for r in rd.values():
                    if r.eng != eng or r.slot is not None or is_dma:
                        deps.append(r)
        best = {}
        for d in deps:
            if d.slot is not None:
                self._wait_on(op, eng, d)
            elif d.eng not in best or d.idx > best[d.eng].idx:
                best[d.eng] = d
        for d in best.values():
            self._wait_on(op, eng, d)
        if is_dma:
            slot.count += 1
            op.slot = slot
            op.slot_count = slot.count
        op.snap = dict(self.seen[eng])
        op.dsnap = dict(self.dseen[eng])
        if not is_dma and op.snap[eng] < op.idx - 1:
            op.snap[eng] = op.idx - 1
        self.ops[eng].append(op)
        for k in reads:
            self.last_r.setdefault(k, {})[eng if not is_dma else ('dma', id(op))] = op
        for k in writes:
            self.last_w[k] = op
            self.last_r[k] = {}
        return op

    def barrier(self):
        lasts = {}
        for e in ENG:
            for op in reversed(self.ops[e]):
                if op.fn is not None and op.slot is None:
                    lasts[e] = op
                    break
        dmas = []
        for e in ENG:
            latest = {}
            for op in self.ops[e]:
                if op.slot is not None:
                    latest[op.slot] = op
            dmas.extend(latest.values())
        for e in ENG:
            op = Op(e, None, len(self.ops[e]))
            for f, d in lasts.items():
                if f != e:
                    self._wait_on(op, e, d)
            for d in dmas:
                self._wait_on(op, e, d)
            op.snap = dict(self.seen[e])
            op.dsnap = dict(self.dseen[e])
            self.ops[e].append(op)

    def finalize(self, nc, es, block):
        self.sems = {}
        for e in ENG:
            c = 0
            for op in self.ops[e]:
                if op.signal:
                    op.count = c
                    c += 1
            nblk = (c + SEM_BLOCK - 1) // SEM_BLOCK
            self.sems[e] = [es.enter_context(nc.semaphore(f"s_{e}_{i}")) for i in range(max(nblk, 1))]
        for s in self.slots:
            if s.count > 0:
                s.sem = es.enter_context(nc.semaphore(f"d_{s.name}"))

        def run(e):
            def body(h):
                for op in self.ops[e]:
                    for w in op.waits:
                        if w[0] == 'dma':
                            h.wait_ge(w[1].sem, 16 * w[2])
                        else:
                            d = w[1]
                            h.wait_ge(self.sems[d.eng][d.count // SEM_BLOCK], d.count % SEM_BLOCK + 1)
                    if op.fn is None:
                        continue
                    ins = op.fn(h)
                    if op.slot is not None:
                        ins.then_inc(op.slot.sem, 16)
                    elif op.signal:
                        ins.then_inc(self.sems[e][op.count // SEM_BLOCK], 1)
            return body

        block.tensor(run('pe'))
        block.scalar(run('act'))
        block.vector(run('dve'))
        block.gpsimd(run('pool'))
        block.sync(run('sp'))


class Builder:
    def __init__(self, nseq, nlayers, stages):
        self.nseq = nseq
        self.nlayers = nlayers
        self.stages = stages
        self.P = Prog()
        self.nc = bass.Bass("TRN2", target_bir_lowering=False)
        self.rot = {}
        self.out_keys = []
        self.slots = {}
        self.ph = 0
        self.apos = 0

    def rr(self, name, n):
        v = self.rot.get(name, 0)
        self.rot[name] = v + 1
        return v % n

    def slot(self, name):
        if name not in self.slots:
            self.slots[name] = self.P.slot(name)
        return self.slots[name]

    def phase(self):
        self.P.barrier()
        self.ph += 1
        self.apos = 0

    def view(self, shape, dt):
        n = 1
        for s_ in shape[1:]:
            n *= s_
        nbytes = n * (4 if dt == F32 else 2)
        nbytes = (nbytes + 63) // 64 * 64
        assert self.apos + nbytes <= ARENA_BYTES, (self.apos, nbytes, shape)
        c0 = self.apos // 4
        ap = self.arena[:, c0:c0 + nbytes // 4]
        self.apos += nbytes
        if dt != F32:
            ap = ap.bitcast(dt)
        ap = ap[:, 0:n]
        if len(shape) == 3:
            ap = ap.rearrange("p (a b) -> p a b", a=shape[1])
        elif len(shape) == 4:
            ap = ap.rearrange("p (a b c) -> p a b c", a=shape[1], b=shape[2])
        return ap

    def mm(self, out, lhsT, rhs, start=True, stop=True, reads=(), writes=()):
        self.P.emit('pe', lambda h: h.matmul(out, lhsT, rhs, start=start, stop=stop), reads, writes)

    def tr(self, out, in_, ident, reads=(), writes=()):
        self.P.emit('pe', lambda h: h.transpose(out, in_, ident), reads, writes)

    def act(self, out, in_, func, reads=(), writes=(), **kw):
        self.P.emit('act', lambda h: h.activation(out=out, in_=in_, func=func, **kw), reads, writes)

    def tt(self, eng, out, in0, in1, op, reads=(), writes=()):
        self.P.emit(eng, lambda h: h.tensor_tensor(out=out, in0=in0, in1=in1, op=op), reads, writes)

    def ts(self, eng, out, in0, s1, op0, s2=None, op1=None, reads=(), writes=()):
        if op1 is None:
            self.P.emit(eng, lambda h: h.tensor_scalar(out=out, in0=in0, scalar1=s1, scalar2=None, op0=op0), reads, writes)
        else:
            self.P.emit(eng, lambda h: h.tensor_scalar(out=out, in0=in0, scalar1=s1, scalar2=s2, op0=op0, op1=op1), reads, writes)

    def stt(self, eng, out, in0, scalar, in1, op0, op1, reads=(), writes=()):
        self.P.emit(eng, lambda h: h.scalar_tensor_tensor(out=out, in0=in0, scalar=scalar, in1=in1, op0=op0, op1=op1), reads, writes)

    def cp(self, eng, out, in_, reads=(), writes=()):
        if eng == 'act':
            self.P.emit('act', lambda h: h.activation(out=out, in_=in_, func=AF.Copy), reads, writes)
        else:
            self.P.emit(eng, lambda h: h.tensor_copy(out=out, in_=in_), reads, writes)

    def rcp(self, out, in_, reads=(), writes=()):
        self.P.emit('dve', lambda h: h.reciprocal(out=out, in_=in_), reads, writes)

    def mset(self, eng, ap, val, writes=()):
        self.P.emit(eng, lambda h: h.memset(ap, val), (), writes)

    def dma(self, eng, out, in_, slotname, reads=(), writes=()):
        self.P.emit(eng, lambda h: h.dma_start(out=out, in_=in_), reads, writes, slot=self.slot(slotname))

    def cst(self, c0, w=128):
        return self.cstt[:, c0:c0 + w]

    def build(self):
        nc = self.nc
        P = self.P
        dr = {}

        def din(name, shape, dt=F32):
            dr[name] = nc.dram_tensor(name, list(shape), dt, kind="ExternalInput").ap()

        nl = self.nlayers
        din('x', [self.nseq, S, D])
        for f in (1, 2):
            din(f'ffn{f}_w_gate', [nl, D, DFF])
            din(f'ffn{f}_w_up', [nl, D, DFF])
            din(f'ffn{f}_w_down', [nl, DFF, D])
        din('w_in', [nl, D, INW])
        din('w_out', [nl, D, D])
        din('gains', [128, 3 * NL + 1, NDC])
        din('cst', [128, CW])
        din('lp', [nl, 128, LPW])
        din('rope', [2, 128, S])
        din('msk', [128, 512])
        self.dr = dr
        self.out = nc.dram_tensor('out', [self.nseq, S, D], F32, kind="ExternalOutput").ap()

        with ExitStack() as es:
            self.es = es

            def sb(name, shape, dt):
                return es.enter_context(nc.sbuf_tensor('sb_' + name, list(shape), dt))

            self.hT = sb('hT', [128, NDC, S], F32)
            self.aT = sb('aT', [128, NDC, S], BF16)
            self.gains = sb('gains', [128, 3 * NL + 1, NDC], F32)
            self.cstt = sb('cst', [128, CW], F32)
            self.ident_b = sb('ident_b', [128, 128], BF16)
            self.onesM = sb('onesM', [128, 128], BF16)
            self.ones_b = sb('ones_b', [128, 128], BF16)
            self.arena = sb('arena', [128, ARENA_BYTES // 4], F32)
            self.ident_f = self.cst(C_ID)
            self.epsc = self.cst(C_EPS, 1)
            self.onec = self.cst(C_ONES, 1)
            self.pbank = [es.enter_context(nc.psum_tensor(f'ps_b{i}', [128, 512], F32)) for i in range(8)]
            block = es.enter_context(nc.Block())

            self.emit_consts()
            for s in range(self.nseq):
                self.emit_load_x(s)
                for l in range(self.nlayers):
                    if 'ffn1' in self.stages:
                        self.emit_ffn(l, 1)
                    if any(st.startswith('mix') for st in self.stages):
                        self.emit_mixer(l)
                    if 'ffn2' in self.stages:
                        self.emit_ffn(l, 2)
                self.emit_final(s)
            P.emit('sp', None, reads=self.out_keys)
            P.finalize(nc, es, block)
        return nc

    def emit_consts(self):
        dr = self.dr
        self.dma('sp', self.gains[:], dr['gains'][:, :, :], 'gains', writes=[('gains',)])
        self.dma('sp', self.cstt[:], dr['cst'][:, :], 'cst', writes=[('cst',)])
        self.cp('dve', self.ident_b[:], self.ident_f, reads=[('cst',)], writes=[('ident_b',)])
        self.mset('dve', self.onesM[:], 1.0 / D, writes=[('onesM',)])
        self.mset('dve', self.ones_b[:], 1.0, writes=[('ones_b',)])

    def emit_load_x(self, s):
        self.phase()
        x = self.dr['x']
        xin = [self.view([128, D], F32) for _ in range(2)]
        for t in range(NCH):
            i = self.rr('xin', 2)
            self.dma('sp', xin[i], x[s, t * 128:(t + 1) * 128, :], f'xin{i}',
                     writes=[('xin', self.ph, i, 0), ('xin', self.ph, i, 1)])
            for half in range(2):
                b = 6 + self.rr('pbT', 2)
                pb = self.pbank[b]
                for q in range(4):
                    dc = half * 4 + q
                    self.tr(pb[:, q * 128:(q + 1) * 128], xin[i][:, dc * 128:(dc + 1) * 128], self.ident_f,
                            reads=[('xin', self.ph, i, half), ('cst',)], writes=[('pb', b)])
                outap = self.hT[:, half * 4:(half + 1) * 4, t * 128:(t + 1) * 128]
                inap = pb[:, :].rearrange("p (q t) -> p q t", q=4)
                self.cp('act' if half == 0 else 'dve', outap, inap, reads=[('pb', b)],
                        writes=K('h', range(half * 4, half * 4 + 4), t))

    def emit_norm(self, tt, out_fn):
        i = self.rr('sq', len(self.sq))
        sq = self.sq[i]
        rstd = self.rstd[i]
        tsl = slice(tt * 512, (tt + 1) * 512)
        hkeys = K('h', range(NDC), range(tt * 4, tt * 4 + 4))
        self.act(sq, self.hT[:, :, tsl], AF.Square, reads=hkeys, writes=[('sq', self.ph, i)])
        pb = self.pbank[5]
        for dc in range(NDC):
            self.mm(pb[:], self.onesM[:], sq[:, dc, :], start=(dc == 0), stop=(dc == NDC - 1),
                    reads=[('sq', self.ph, i), ('onesM',)], writes=[('pb', 5)])
        self.act(rstd, pb[:], AF.Sqrt, bias=self.epsc, reads=[('pb', 5), ('cst',)], writes=[('rstd', self.ph, i)])
        self.rcp(rstd, rstd, reads=[('rstd', self.ph, i)], writes=[('rstd', self.ph, i)])
        for dc in range(NDC):
            out_fn(dc, rstd, ('rstd', self.ph, i), tsl, tt)

    def alloc_norm(self, n=2):
        self.sq = [self.view([128, NDC, 512], BF16) for _ in range(n)]
        self.rstd = [self.view([128, 512], F32) for _ in range(n)]

    def emit_norm_to_aT(self, gidx):
        for tt in range(4):
            def out_fn(dc, rstd, rkey, tsl, tt):
                self.stt('dve', self.aT[:, dc, tsl], self.hT[:, dc, tsl], self.gains[:, gidx, dc:dc + 1], rstd,
                         ALU.mult, ALU.mult,
                         reads=K('h', dc, range(tt * 4, tt * 4 + 4)) + [rkey, ('gains',)],
                         writes=K('a', dc, range(tt * 4, tt * 4 + 4)))
            self.emit_norm(tt, out_fn)

    def emit_final(self, s):
        self.phase()
        self.alloc_norm(2)
        xo_ = [self.view([128, D], F32) for _ in range(2)]
        gidx = 3 * NL
        for tt in range(4):
            def out_fn(dc, rstd, rkey, tsl, tt):
                hk = K('h', dc, range(tt * 4, tt * 4 + 4))
                self.stt('dve', self.hT[:, dc, tsl], self.hT[:, dc, tsl], self.gains[:, gidx, dc:dc + 1], rstd,
                         ALU.mult, ALU.mult, reads=hk + [rkey, ('gains',)], writes=hk)
            self.emit_norm(tt, out_fn)
        for t in range(NCH):
            i = self.rr('xo', 2)
            xo = xo_[i]
            for half in range(2):
                b = 6 + self.rr('pbT', 2)
                pb = self.pbank[b]
                for q in range(4):
                    dc = half * 4 + q
                    self.tr(pb[:, q * 128:(q + 1) * 128], self.hT[:, dc, t * 128:(t + 1) * 128], self.ident_f,
                            reads=[('h', dc, t), ('cst',)], writes=[('pb', b)])
                self.cp('act' if half == 0 else 'dve', xo[:, half * 512:(half + 1) * 512], pb[:],
                        reads=[('pb', b)], writes=[('xo', self.ph, i, half)])
            self.dma('sp', self.out[s, t * 128:(t + 1) * 128, :], xo, f'xout{i}',
                     reads=[('xo', self.ph, i, 0), ('xo', self.ph, i, 1)], writes=[('out_dram', s, t)])
            self.out_keys.append(('out_dram', s, t))

    def emit_ffn(self, l, which):
        self.phase()
        ph = self.ph
        dr = self.dr
        GF = 4
        self.alloc_norm(2)
        wg_ = [self.view([128, NDC, GF * 128], BF16) for _ in range(2)]
        wu_ = [self.view([128, NDC, GF * 128], BF16) for _ in range(2)]
        wd_ = [self.view([128, GF, D], BF16) for _ in range(2)]
        hid_ = [self.view([128, GF, S], BF16) for _ in range(2)]
        sg_ = [self.view([128, 512], BF16) for _ in range(2)]
        self.emit_norm_to_aT(3 * l + (0 if which == 1 else 2))
        Wg = dr[f'ffn{which}_w_gate'][l].rearrange("(dc p) f -> p dc f", p=128)
        Wu = dr[f'ffn{which}_w_up'][l].rearrange("(dc p) f -> p dc f", p=128)
        Wd = dr[f'ffn{which}_w_down'][l].rearrange("(fc p) d -> p fc d", p=128)
        groups = [(g0, min(GF, NFC - g0)) for g0 in range(0, NFC, GF)]
        for (fc0, nfc) in groups:
            ws = self.rr('wslot', 2)
            wg, wu, wd = wg_[ws], wu_[ws], wd_[ws]
            self.dma('pool', wg[:, :, 0:nfc * 128], Wg[:, :, fc0 * 128:(fc0 + nfc) * 128], f'wg{ws}', writes=[('wg', ph, ws)])
            self.dma('pool', wu[:, :, 0:nfc * 128], Wu[:, :, fc0 * 128:(fc0 + nfc) * 128], f'wu{ws}', writes=[('wu', ph, ws)])
            self.dma('pool', wd[:, 0:nfc, :], Wd[:, fc0:fc0 + nfc, :], f'wd{ws}', writes=[('wd', ph, ws)])
            hs = self.rr('hslot', 2)
            hid = hid_[hs]
            for fci in range(nfc):
                for tt in range(4):
                    tsl = slice(tt * 512, (tt + 1) * 512)
                    akeys = K('a', range(NDC), range(tt * 4, tt * 4 + 4))
                    bi = self.rr('pbG', 2)
                    pg, pu = self.pbank[bi], self.pbank[2 + bi]
                    for dc in range(NDC):
                        self.mm(pg[:], wg[:, dc, fci * 128:(fci + 1) * 128], self.aT[:, dc, tsl],
                                start=(dc == 0), stop=(dc == NDC - 1),
                                reads=[('wg', ph, ws)] + akeys, writes=[('pb', bi)])
                    for dc in range(NDC):
                        self.mm(pu[:], wu[:, dc, fci * 128:(fci + 1) * 128], self.aT[:, dc, tsl],
                                start=(dc == 0), stop=(dc == NDC - 1),
                                reads=[('wu', ph, ws)] + akeys, writes=[('pb', 2 + bi)])
                    si = self.rr('sg', 2)
                    sg = sg_[si]
                    self.act(sg, pg[:], AF.Silu, reads=[('pb', bi)], writes=[('sg', ph, si)])
                    self.tt('dve', hid[:, fci, tsl], pu[:], sg, ALU.mult,
                            reads=[('pb', 2 + bi), ('sg', ph, si)], writes=[('hid', ph, hs, fci, tt)])
            for dc in range(NDC):
                for tt in range(4):
                    tsl = slice(tt * 512, (tt + 1) * 512)
                    bi = (4, 6, 7)[self.rr('pbD', 3)]
                    pd = self.pbank[bi]
                    for fci in range(nfc):
                        self.mm(pd[:], wd[:, fci, dc * 128:(dc + 1) * 128], hid[:, fci, tsl],
                                start=(fci == 0), stop=(fci == nfc - 1),
                                reads=[('wd', ph, ws), ('hid', ph, hs, fci, tt)], writes=[('pb', bi)])
                    hk = K('h', dc, range(tt * 4, tt * 4 + 4))
                    self.stt('dve', self.hT[:, dc, tsl], pd[:], 0.5, self.hT[:, dc, tsl], ALU.mult, ALU.add,
                             reads=[('pb', bi)] + hk, writes=hk)

    def emit_mixer(self, l):
        self.phase()
        self.alloc_norm(2)
        self.emit_norm_to_aT(3 * l + 1)
        if 'mix' in self.stages or 'mixm' in self.stages:
            self.emit_lin_group(l, 'm')
        if 'mix' in self.stages or 'mixr' in self.stages:
            self.emit_lin_group(l, 'r')
        if 'mix' in self.stages or 'mixg' in self.stages:
            self.emit_gdn(l)

    def load_lp(self, l):
        lp = self.view([128, LPW], F32)
        self.dma('sp', lp, self.dr['lp'][l], 'lp', writes=[('lp', self.ph)])
        return lp

    def gate_cumsum(self, LF, lfkey, CG, cgkey):
        pb = self.pbank[5]
        pv = pb[:, 0:NCH * 8].rearrange("p (c e) -> p c e", c=NCH)
        for c in range(NCH):
            self.mm(pv[:, c, 0:4], self.cst(C_TRIU), LF[:, c, :], reads=[lfkey, ('cst',)], writes=[('pb', 5)])
            self.mm(pv[:, c, 4:8], self.cst(C_ONES), LF[:, c, :], reads=[lfkey, ('cst',)], writes=[('pb', 5)])
        self.cp('act', CG, pv, reads=[('pb', 5)], writes=[cgkey])

    def head_post(self, num, numkeys, nh, dh, gain, gate_c, gatekey, ytok_c, ykey, scr):
        ph = self.ph
        SQ, SS, RS, T1 = scr
        i = self.rr('hp', 2)
        sqv = SQ[i][:, 0:nh * dh].rearrange("p (h e) -> p h e", h=nh)
        t1v = T1[i][:, 0:nh * dh].rearrange("p (h e) -> p h e", h=nh)
        ss = SS[i][:, 0:nh]
        rs = RS[i][:, 0:nh]
        self.tt('pool', sqv, num, num, ALU.mult, reads=numkeys, writes=[('hpsq', ph, i)])
        self.P.emit('dve', lambda h: h.reduce_sum(out=ss, in_=sqv, axis=AX.X),
                    reads=[('hpsq', ph, i)], writes=[('hpss', ph, i)])
        self.act(rs, ss, AF.Ln, bias=self.epsc, scale=1.0 / dh, reads=[('hpss', ph, i), ('cst',)], writes=[('hprs', ph, i)])
        self.act(rs, rs, AF.Exp, scale=-0.5, reads=[('hprs', ph, i)], writes=[('hprs', ph, i)])
        self.tt('dve', t1v, num, rs.unsqueeze(2).to_broadcast([128, nh, dh]), ALU.mult,
                reads=numkeys + [('hprs', ph, i)], writes=[('hpt1', ph, i)])
        self.tt('pool', t1v, t1v, gain, ALU.mult, reads=[('hpt1', ph, i), ('lp', ph)], writes=[('hpt1', ph, i)])
        self.tt('dve', ytok_c, t1v, gate_c, ALU.mult, reads=[('hpt1', ph, i), gatekey], writes=[ykey])

    def alloc_post(self, width):
        SQ = [self.view([128, width], F32) for _ in range(2)]
        SS = [self.view([128, 4], F32) for _ in range(2)]
        RS = [self.view([128, 4], F32) for _ in range(2)]
        T1 = [self.view([128, width], F32) for _ in range(2)]
        return (SQ, SS, RS, T1)

    def emit_outproj(self, l, m0, width, ytok, yT, Wo):
        ph = self.ph
        nmc = width // 128
        Wod = self.dr['w_out'][l].rearrange("(mc p) d -> p mc d", p=128)
        self.dma('pool', Wo[:, 0:nmc, :], Wod[:, m0 // 128:m0 // 128 + nmc, :], 'wo', writes=[('wo', ph)])
        for c in range(NCH):
            b = 6 + self.rr('pbT', 2)
            pbv = self.pbank[b][:].bitcast(BF16)[:, 0:nmc * 128].rearrange("p (m t) -> p m t", m=nmc)
            for mc in range(nmc):
                self.tr(pbv[:, mc, :], ytok[:, c, mc * 128:(mc + 1) * 128], self.ident_b[:],
                        reads=[('ytok', ph, c), ('ident_b',)], writes=[('pb', b)])
            self.cp('act' if c % 2 == 0 else 'dve', yT[:, 0:nmc, c * 128:(c + 1) * 128], pbv,
                    reads=[('pb', b)], writes=[('yT', ph, c)])
        for dc in range(NDC):
            for tt in range(4):
                tsl = slice(tt * 512, (tt + 1) * 512)
                bi = (4, 6, 7)[self.rr('pbD', 3)]
                pd = self.pbank[bi]
                for mc in range(nmc):
                    self.mm(pd[:], Wo[:, mc, dc * 128:(dc + 1) * 128], yT[:, mc, tsl],
                            start=(mc == 0), stop=(mc == nmc - 1),
                            reads=[('wo', ph)] + K('yT', ph, range(tt * 4, tt * 4 + 4)), writes=[('pb', bi)])
                hk = K('h', dc, range(tt * 4, tt * 4 + 4))
                self.tt('dve', self.hT[:, dc, tsl], pd[:], self.hT[:, dc, tsl], ALU.add,
                        reads=[('pb', bi)] + hk, writes=hk)

    def decay_mats(self, LA, lakey, c, h, want_T, want_S, pT, pS, bT, bS, rt):
        ph = self.ph
        ri = self.rr('rt', len(rt))
        r = rt[ri]
        self.ts('dve', r, self.cst(C_TRIU), LA[:, c, h:h + 1], ALU.mult, reads=[lakey, ('cst',)], writes=[('rt', ph, ri)])
        if want_T:
            self.mm(pT, self.cst(C_MGT), r, start=True, stop=False, reads=[('rt', ph, ri), ('cst',)], writes=[('pb', bT)])
            self.mm(pT, self.ident_f, self.cst(C_NEGT), start=False, stop=True, reads=[('cst',)], writes=[('pb', bT)])
        if want_S:
            self.mm(pS, r, self.cst(C_MGT), start=True, stop=False, reads=[('rt', ph, ri), ('cst',)], writes=[('pb', bS)])
            self.mm(pS, self.ident_f, self.cst(C_NEGS), start=False, stop=True, reads=[('cst',)], writes=[('pb', bS)])

    def emit_lin_group(self, l, kind):
        self.phase()
        ph = self.ph
        dr = self.dr
        is_m = kind == 'm'
        col0 = 0 if is_m else 1032
        m0 = 0 if is_m else 256
        E = 65 if is_m else 64
        lp = self.load_lp(l)
        W = self.view([128, NDC, 1536], BF16)
        qkT = self.view([128, 6, S], BF16)
        vtok = self.view([128, NCH, 4, E], BF16)
        gate = self.view([128, NCH, 256], BF16)
        ytok = gate
        G = self.view([128, NCH, 8], F32)
        CG = self.view([128, NCH, 8], F32)
        LF = self.view([128, NCH, 4], F32)
        QS = self.view([128, NCH, 4], F32)
        KS2 = self.view([128, NCH, 4], F32)
        DEC = self.view([128, NCH, 4], F32)
        TMP = self.view([128, NCH, 4], F32)
        C32 = self.view([128, 2, E], F32)
        Cb = self.view([128, 2, E], BF16)
        KL = 3
        wT_ = [self.view([128, 4, 128], F32) for _ in range(KL)] if is_m else None
        rt4_ = [[self.view([128, 128], F32) for _ in range(4)] for _ in range(KL)] if is_m else None
        scm_ = [self.view([128, 4, 128], BF16) for _ in range(KL)]
        khat_ = [self.view([128, 4, 64], BF16) for _ in range(KL)]
        T2_ = [self.view([128, 4, E], F32) for _ in range(KL)]
        NUM_ = [self.view([128, 4, E], F32) for _ in range(KL)]
        HN_ = [self.view([128, 4, 64], F32) for _ in range(KL)] if is_m else None
        R_ = [self.view([128, 4], F32) for _ in range(KL)]
        rt = [self.view([128, 128], F32) for _ in range(2)]
        post = self.alloc_post(256)
        Wd = dr['w_in'][l].rearrange("(dc p) f -> p dc f", p=128)
        self.dma('pool', W[:, :, 0:(1032 if is_m else 1024)], Wd[:, :, col0:col0 + (1032 if is_m else 1024)], 'wgrp',
                 writes=[('W', ph)])
        if not is_m:
            ROPE = [self.view([128, S], BF16) for _ in range(2)]
            rtmp = [self.view([128, 512], F32) for _ in range(2)]
            rtmpB = [self.view([128, 512], F32) for _ in range(2)]
            for which in range(2):
                for two in range(2):
                    src = W[:, :, which * 256:(which + 1) * 256].rearrange(
                        "p dc (h two f) -> p dc h two f", h=4, two=2)[:, :, :, two, :]
                    dst = W[:, :, 1024 + which * 256:1024 + (which + 1) * 256].rearrange(
                        "p dc (h two f) -> p dc h two f", h=4, two=2)[:, :, :, 1 - two, :]
                    self.cp('act' if two == 0 else 'dve', dst, src, reads=[('W', ph)], writes=[('Wsw', ph)])
            self.dma('pool', ROPE[0], dr['rope'][0], 'rope', writes=[('rope', ph)])
            self.dma('pool', ROPE[1], dr['rope'][1], 'rope', writes=[('rope', ph)])

        self.mset('dve', qkT[:, 0:4, :], 0.0, writes=[('qz', ph)])
        for cc in range(4):
            for tt in range(4):
                tsl = slice(tt * 512, (tt + 1) * 512)
                akeys = K('a', range(NDC), range(tt * 4, tt * 4 + 4))
                bi = self.rr('pbG', 2)
                pa = self.pbank[bi]
                for dc in range(NDC):
                    self.mm(pa[:], W[:, dc, cc * 128:(cc + 1) * 128], self.aT[:, dc, tsl],
                            start=(dc == 0), stop=(dc == NDC - 1), reads=[('W', ph)] + akeys, writes=[('pb', bi)])
                if cc < 2:
                    dsts = [(slice(0, 64), 2 * cc), (slice(64, 128), 2 * cc + 1)]
                else:
                    dsts = [(slice(0, 128), 4 + cc - 2)]
                if is_m:
                    for n_, (psl, qi) in enumerate(dsts):
                        self.cp('act' if n_ == 0 else 'dve', qkT[psl, qi, tsl], pa[psl, :], reads=[('pb', bi), ('qz', ph)],
                                writes=K('qkT', ph, qi, range(tt * 4, tt * 4 + 4)))
                else:
                    pb2 = self.pbank[2 + bi]
                    for dc in range(NDC):
                        self.mm(pb2[:], W[:, dc, 1024 + cc * 128:1024 + (cc + 1) * 128], self.aT[:, dc, tsl],
                                start=(dc == 0), stop=(dc == NDC - 1), reads=[('Wsw', ph)] + akeys, writes=[('pb', 2 + bi)])
                    ri = self.rr('rtmp', 2)
                    self.tt('dve', rtmp[ri], pa[:], ROPE[0][:, tsl], ALU.mult, reads=[('pb', bi), ('rope', ph)],
                            writes=[('rtmp', ph, ri)])
                    self.tt('dve', rtmpB[ri], pb2[:], ROPE[1][:, tsl], ALU.mult, reads=[('pb', 2 + bi), ('rope', ph)],
                            writes=[('rtmpB', ph, ri)])
                    for (psl, qi) in dsts:
                        self.tt('pool', qkT[psl, qi, tsl], rtmp[ri][psl, :], rtmpB[ri][psl, :], ALU.add,
                                reads=[('rtmp', ph, ri), ('rtmpB', ph, ri), ('qz', ph)],
                                writes=K('qkT', ph, qi, range(tt * 4, tt * 4 + 4)))
        if is_m:
            self.mset('dve', vtok[:, :, :, 64:65], 1.0, writes=[('vone', ph)])
        for c in range(NCH):
            csl = slice(c * 128, (c + 1) * 128)
            akeys = K('a', range(NDC), c)
            bi = self.rr('pbG', 2)
            px = self.pbank[bi]
            py = self.pbank[2 + bi]
            for dc in range(NDC):
                self.mm(px[:], self.aT[:, dc, csl], W[:, dc, 512:1024], start=(dc == 0), stop=(dc == NDC - 1),
                        reads=[('W', ph)] + akeys, writes=[('pb', bi)])
            if is_m:
                for dc in range(NDC):
                    self.mm(py[:, 0:8], self.aT[:, dc, csl], W[:, dc, 1024:1032], start=(dc == 0), stop=(dc == NDC - 1),
                            reads=[('W', ph)] + akeys, writes=[('pb', 2 + bi)])
                self.tt('dve', G[:, c, :], py[:, 0:8], lp[:, 0:8], ALU.add, reads=[('pb', 2 + bi), ('lp', ph)],
                        writes=[('G', ph, c)])
            self.cp('act', vtok[:, c, :, 0:64], px[:, 0:256].rearrange("p (h e) -> p h e", h=4),
                    reads=[('pb', bi), ('vone', ph)] if is_m else [('pb', bi)], writes=[('vtok', ph, c)])
            self.act(gate[:, c, :], px[:, 256:512], AF.Sigmoid if is_m else AF.Silu, reads=[('pb', bi)],
                     writes=[('gate', ph, c)])
        STOP = os.environ.get('LIN_STOP', '')
        if STOP == 'B':
            return
        gk = [('G', ph, c) for c in range(NCH)]
        if is_m:
            self.act(TMP, G[:, :, 4:8], AF.Exp, scale=-1.0, reads=gk, writes=[('TMP', ph)])
            self.act(TMP, TMP, AF.Ln, bias=self.onec, reads=[('TMP', ph), ('cst',)], writes=[('TMP', ph)])
            self.ts('dve', LF, TMP, -1.0, ALU.mult, reads=[('TMP', ph)], writes=[('LF', ph)])
            self.gate_cumsum(LF, ('LF', ph), CG, ('CG', ph))
            self.act(QS, CG[:, :, 0:4], AF.Exp, reads=[('CG', ph)], writes=[('QS', ph)])
            self.act(DEC, CG[:, :, 4:8], AF.Exp, reads=[('CG', ph)], writes=[('DEC', ph)])
            self.tt('dve', TMP, CG[:, :, 4:8], CG[:, :, 0:4], ALU.subtract, reads=[('CG', ph), ('TMP', ph)], writes=[('TMP', ph)])
            self.tt('dve', TMP, TMP, G[:, :, 0:4], ALU.add, reads=[('TMP', ph)] + gk, writes=[('TMP', ph)])
            self.act(KS2, TMP, AF.Exp, reads=[('TMP', ph)], writes=[('KS2', ph)])
            self.ts('dve', KS2, KS2, 0.125, ALU.mult, reads=[('KS2', ph)], writes=[('KS2', ph)])
        else:
            self.cp('dve', QS, self.cst(C_RQS, 4).unsqueeze(1).to_broadcast([128, NCH, 4]), reads=[('cst',)], writes=[('QS', ph)])
            self.cp('dve', KS2, self.cst(C_RKS, 4).unsqueeze(1).to_broadcast([128, NCH, 4]), reads=[('cst',)], writes=[('KS2', ph)])
            self.cp('dve', DEC, self.cst(C_RDEC, 4).unsqueeze(1).to_broadcast([128, NCH, 4]), reads=[('cst',)], writes=[('DEC', ph)])
        if STOP == 'C':
            return
        self.mset('dve', C32, 0.0, writes=[('C32', ph)])
        self.mset('dve', Cb, 0.0, writes=[('Cb', ph)])
        gain = lp[:, 16 + m0:16 + m0 + 256].rearrange("p (h e) -> p h e", h=4)
        def lin_chunk(c, wi):
                csl = slice(c * 128, (c + 1) * 128)
                if is_m:
                    wT = wT_[wi]
                    bw = (4, 6)[self.rr('pbW', 2)]
                    pw = self.pbank[bw][:].rearrange("p (h i) -> p h i", h=4)
                    for h in range(4):
                        self.ts('dve', rt4_[wi][h], self.cst(C_TRIU), LF[:, c, h:h + 1], ALU.mult, reads=[('LF', ph), ('cst',)],
                                writes=[('rt4', ph, wi, h)])
                    yield
                    for h in range(4):
                        self.mm(pw[:, h, :], self.cst(C_MGT), rt4_[wi][h], start=True, stop=False,
                                reads=[('rt4', ph, wi, h), ('cst',)], writes=[('pb', bw)])
                        self.mm(pw[:, h, :], self.ident_f, self.cst(C_NEGT), start=False, stop=True, reads=[('cst',)], writes=[('pb', bw)])
                    for h in range(4):
                        self.act(wT[:, h, :], pw[:, h, :], AF.Exp, bias=G[:, c, h:h + 1], reads=[('pb', bw), ('G', ph, c)],
                                 writes=[('wT', ph, wi)])
                    wkey = [('wT', ph, wi)]
                else:
                    wT = self.cst(C_RW, 512).rearrange("p (h i) -> p h i", h=4)
                    wkey = [('cst',)]
                yield
                bs = self.rr('pbG', 2)
                psc = self.pbank[bs][:].rearrange("p (h i) -> p h i", h=4)
                for h in range(4):
                    self.mm(psc[:, h, :], qkT[:, 4 + h // 2, csl], qkT[:, h, csl],
                            reads=[('qkT', ph, 4 + h // 2, c), ('qkT', ph, h, c)], writes=[('pb', bs)])
                scm = scm_[wi]
                self.stt('dve', scm, psc, 0.125, wT, ALU.mult, ALU.mult, reads=[('pb', bs)] + wkey, writes=[('scm', ph, wi)])
                if STOP == 'D1':
                    return
                bk = 7
                pkt = self.pbank[bk][:].bitcast(BF16)[:, 0:256]
                for hh in range(2):
                    self.tr(pkt[:, hh * 128:(hh + 1) * 128], qkT[:, 4 + hh, csl], self.ident_b[:],
                            reads=[('qkT', ph, 4 + hh, c), ('ident_b',)], writes=[('pb', bk)])
                khat = khat_[wi]
                self.tt('dve', khat, pkt.rearrange("p (h e) -> p h e", h=4),
                        KS2[:, c, :].unsqueeze(2).to_broadcast([128, 4, 64]), ALU.mult,
                        reads=[('pb', bk), ('KS2', ph)], writes=[('khat', ph, wi)])
                if STOP == 'D2':
                    return
                yield
                bo = 2 + self.rr('pbO', 2)
                po1 = self.pbank[bo][:, 0:4 * E].rearrange("p (h e) -> p h e", h=4)
                bo2 = (4, 6)[self.rr('pbW', 2)]
                po2 = self.pbank[bo2][:, 0:4 * E].rearrange("p (h e) -> p h e", h=4)
                for h in range(4):
                    self.mm(po1[:, h, :], scm[:, h, :], vtok[:, c, h, :], reads=[('scm', ph, wi), ('vtok', ph, c)],
                            writes=[('pb', bo)])
                    self.mm(po2[:, h, :], qkT[:, h, csl], Cb[:, h // 2, :], reads=[('qkT', ph, h, c), ('Cb', ph)],
                            writes=[('pb', bo2)])
                T2, NUM = T2_[wi], NUM_[wi]
                self.tt('dve', T2, po2, QS[:, c, :].unsqueeze(2).to_broadcast([128, 4, E]), ALU.mult,
                        reads=[('pb', bo2), ('QS', ph)], writes=[('T2', ph, wi)])
                self.tt('dve', NUM, T2, po1, ALU.add, reads=[('T2', ph, wi), ('pb', bo)], writes=[('NUM', ph, wi)])
                if STOP == 'D3':
                    return
                if is_m:
                    R, HN = R_[wi], HN_[wi]
                    self.stt('dve', R, NUM[:, :, 64], -1.0, NUM[:, :, 64], ALU.mult, ALU.max, reads=[('NUM', ph, wi)], writes=[('R', ph, wi)])
                    self.ts('dve', R, R, 1.0, ALU.max, reads=[('R', ph, wi)], writes=[('R', ph, wi)])
                    self.rcp(R, R, reads=[('R', ph, wi)], writes=[('R', ph, wi)])
                    self.tt('dve', HN, NUM[:, :, 0:64], R.unsqueeze(2).to_broadcast([128, 4, 64]), ALU.mult,
                            reads=[('NUM', ph, wi), ('R', ph, wi)], writes=[('HN', ph, wi)])
                    hsrc, hkeys = HN, [('HN', ph, wi)]
                else:
                    hsrc, hkeys = NUM, [('NUM', ph, wi)]
                yield
                if STOP == 'D4':
                    return
                for hh in range(2):
                    bc = (4, 6)[self.rr('pbW', 2)]
                    pc = self.pbank[bc][:, 0:2 * E]
                    self.mm(pc, khat[:, 2 * hh:2 * hh + 2, :].rearrange("p h e -> p (h e)"),
                            vtok[:, c, 2 * hh:2 * hh + 2, :].rearrange("p h e -> p (h e)"),
                            reads=[('khat', ph, wi), ('vtok', ph, c)], writes=[('pb', bc)])
                    for q in range(2):
                        h = 2 * hh + q
                        hp = slice(q * 64, q * 64 + 64)
                        self.stt('dve', C32[hp, hh, :], C32[hp, hh, :], DEC[hp, c, h:h + 1], pc[hp, q * E:(q + 1) * E],
                                 ALU.mult, ALU.add, reads=[('C32', ph), ('DEC', ph), ('pb', bc)], writes=[('C32', ph)])
                self.cp('act', Cb, C32, reads=[('C32', ph)], writes=[('Cb', ph)])
                yield
                self.head_post(hsrc, hkeys, 4, 64, gain, gate[:, c, :].rearrange("p (h e) -> p h e", h=4), ('gate', ph, c),
                               ytok[:, c, :].rearrange("p (h e) -> p h e", h=4), ('gate', ph, c), post)

        active = []
        nxt = 0
        rounds = 0
        while nxt < NCH or active:
            if nxt < NCH and len(active) < KL and rounds % 2 == 0:
                active.append(lin_chunk(nxt, nxt % KL))
                nxt += 1
            for g_ in list(active):
                try:
                    next(g_)
                except StopIteration:
                    active.remove(g_)
            rounds += 1
        if STOP:
            return
        self.phase()
        self.view([128, LPW], F32)
        self.view([128, NDC, 1536], BF16)
        yT = self.view([128, 6, S], BF16)[:, 0:4, :]
        self.view([128, NCH, 4, E], BF16)
        ytok2 = self.view([128, NCH, 256], BF16)
        Wo = self.view([128, 4, D], BF16)
        for c in range(NCH):
            self.P.last_w[('ytok', self.ph, c)] = self.P.last_w.get(('gate', ph, c))
            self.P.last_r[('ytok', self.ph, c)] = {}
        self.emit_outproj(l, m0, 256, ytok2, yT, Wo)

    def emit_gdn(self, l):
        self.phase()
        ph = self.ph
        dr = self.dr
        Wd = dr['w_in'][l].rearrange("(dc p) f -> p dc f", p=128)
        lp = self.load_lp(l)
        ZS = self.view([128, NCH, 512], BF16)
        ytok = ZS
        Wz = self.view([128, NDC, 520], BF16)
        Wh = Wz[:, :, 0:384]
        ubase = self.apos
        RAW = self.view([128, 3, S + 4], BF16)
        ACC = self.view([128, S], F32)
        SQ = self.view([128, S], BF16)
        RN = [self.view([128, 512], F32) for _ in range(1)]
        uend1 = self.apos
        self.apos = ubase
        KI = 5
        TS = []
        for _ in range(KI):
            T = {}
            for nm in ('rt', 'dT', 'dS', 'N32', 'NbA', 'NbB', 'MbA', 'MbB', 'YA', 'YB', 'NUM', 'XT', 'A1'):
                T[nm] = self.view([128, 128], F32)
            T['CL'] = self.view([128, 3, 128], F32)
            for nm in ('rk', 'kh', 'bv', 'QKT', 'X', 'nWk', 'U'):
                T[nm] = self.view([128, 128], BF16)
            TS.append(T)
        SH = {nm: [self.view([128, 128], F32) for _ in range(2)] for nm in ('M32', 'T2')}
        self.apos = max(uend1, self.apos)
        CV = self.view([128, 3, S], BF16)
        GG = self.view([128, NCH, 8], F32)
        CG = self.view([128, NCH, 8], F32)
        LA = self.view([128, NCH, 4], F32)
        BETA = self.view([128, NCH, 4], F32)
        BEG = self.view([128, NCH, 4], F32)
        KH = self.view([128, NCH, 4], F32)
        DEC = self.view([128, NCH, 4], F32)
        QSG = self.view([128, NCH, 4], F32)
        TMP = self.view([128, NCH, 4], F32)
        NA = self.view([128, 4], F32)
        MSK = self.view([128, 4, 128], F32)
        S32 = self.view([128, 128], F32)
        Sb = self.view([128, 128], BF16)
        post = self.alloc_post(128)
        gcol = 2056
        self.dma('sp', MSK, dr['msk'].rearrange("p (a b) -> p a b", a=4), 'msk', writes=[('MSK', ph)])
        self.dma('pool', Wz[:, :, 0:520], Wd[:, :, 3592:4112], 'wz', writes=[('Wz', ph)])
        for c in range(NCH):
            csl = slice(c * 128, (c + 1) * 128)
            akeys = K('a', range(NDC), c)
            bi = self.rr('pbG', 2)
            px, py = self.pbank[bi], self.pbank[2 + bi]
            for dc in range(NDC):
                self.mm(px[:], self.aT[:, dc, csl], Wz[:, dc, 0:512], start=(dc == 0), stop=(dc == NDC - 1),
                        reads=[('Wz', ph)] + akeys, writes=[('pb', bi)])
            for dc in range(NDC):
                self.mm(py[:, 0:8], self.aT[:, dc, csl], Wz[:, dc, 512:520], start=(dc == 0), stop=(dc == NDC - 1),
                        reads=[('Wz', ph)] + akeys, writes=[('pb', 2 + bi)])
            self.act(ZS[:, c, :], px[:], AF.Silu, reads=[('pb', bi)], writes=[('ZS', ph, c)])
            self.cp('dve', GG[:, c, :], py[:, 0:8], reads=[('pb', 2 + bi)], writes=[('GG', ph, c)])
        ggk = [('GG', ph, c) for c in range(NCH)]
        self.tt('dve', TMP, GG[:, :, 0:4], lp[:, 12:16].unsqueeze(1).to_broadcast([128, NCH, 4]), ALU.add,
                reads=ggk + [('lp', ph)], writes=[('TMP', ph)])
        self.act(TMP, TMP, AF.Exp, reads=[('TMP', ph)], writes=[('TMP', ph)])
        self.act(TMP, TMP, AF.Ln, bias=self.onec, reads=[('TMP', ph), ('cst',)], writes=[('TMP', ph)])
        self.act(NA, lp[:, 8:12], AF.Exp, reads=[('lp', ph)], writes=[('NA', ph)])
        self.stt('dve', LA, TMP, -1.0, NA.unsqueeze(1).to_broadcast([128, NCH, 4]), ALU.mult, ALU.mult,
                 reads=[('TMP', ph), ('NA', ph)], writes=[('LA', ph)])
        self.act(BETA, GG[:, :, 4:8], AF.Sigmoid, reads=ggk, writes=[('BETA', ph)])
        self.gate_cumsum(LA, ('LA', ph), CG, ('CG', ph))
        self.act(QSG, CG[:, :, 0:4], AF.Exp, reads=[('CG', ph)], writes=[('QSG', ph)])
        self.tt('dve', BEG, QSG, BETA, ALU.mult, reads=[('QSG', ph), ('BETA', ph)], writes=[('BEG', ph)])
        self.ts('dve', QSG, QSG, 128.0 ** -0.5, ALU.mult, reads=[('QSG', ph), ('BEG', ph)], writes=[('QSG', ph)])
        self.act(DEC, CG[:, :, 4:8], AF.Exp, reads=[('CG', ph)], writes=[('DEC', ph)])
        self.tt('dve', TMP, CG[:, :, 4:8], CG[:, :, 0:4], ALU.subtract, reads=[('CG', ph), ('TMP', ph)], writes=[('TMP', ph)])
        self.act(KH, TMP, AF.Exp, reads=[('TMP', ph)], writes=[('KH', ph)])

        def load_wh(hh):
            for x3 in range(3):
                self.dma('pool', Wh[:, :, x3 * 128:(x3 + 1) * 128],
                         Wd[:, :, gcol + x3 * 512 + hh * 128:gcol + x3 * 512 + (hh + 1) * 128], 'wh', writes=[('Wz', ph)])

        for h in range(4):
            self.mset('dve', RAW[:, :, 0:3], 0.0, writes=[('rawpad', ph)])
            if h == 0:
                load_wh(0)
            for x3 in range(3):
                for tt in range(4):
                    tsl = slice(tt * 512, (tt + 1) * 512)
                    akeys = K('a', range(NDC), range(tt * 4, tt * 4 + 4))
                    bi = self.rr('pbG', 2)
                    pa = self.pbank[bi]
                    for dc in range(NDC):
                        self.mm(pa[:], Wh[:, dc, x3 * 128:(x3 + 1) * 128], self.aT[:, dc, tsl],
                                start=(dc == 0), stop=(dc == NDC - 1), reads=[('Wz', ph)] + akeys, writes=[('pb', bi)])
                    self.cp('act', RAW[:, x3, 3 + tt * 512:3 + (tt + 1) * 512], pa[:], reads=[('pb', bi), ('rawpad', ph)],
                            writes=[('RAW', ph, x3, tt)])
                if x3 == 2 and h < 3:
                    load_wh(h + 1)
                rawk = K('RAW', ph, x3, range(4))
                cw0 = 16 + 1024 + (x3 * 4 + h) * 4
                self.ts('dve', ACC, RAW[:, x3, 0:S], lp[:, cw0:cw0 + 1], ALU.mult, reads=rawk + [('lp', ph)], writes=[('ACC', ph)])
                for tap in range(1, 4):
                    self.stt('dve', ACC, RAW[:, x3, tap:tap + S], lp[:, cw0 + tap:cw0 + tap + 1], ACC, ALU.mult, ALU.add,
                             reads=rawk + [('lp', ph), ('ACC', ph)], writes=[('ACC', ph)])
                self.act(CV[:, x3, :], ACC, AF.Silu, reads=[('ACC', ph)], writes=K('CV', ph, x3, range(4)))
                if x3 < 2:
                    self.act(SQ, CV[:, x3, :], AF.Square, reads=K('CV', ph, x3, range(4)), writes=[('SQ', ph)])
                    for tt in range(4):
                        tsl = slice(tt * 512, (tt + 1) * 512)
                        pn = self.pbank[5]
                        self.mm(pn[:], self.ones_b[:], SQ[:, tsl], reads=[('SQ', ph), ('ones_b',)], writes=[('pb', 5)])
                        ri = 0
                        self.act(RN[ri], pn[:], AF.Sqrt, bias=self.epsc, reads=[('pb', 5), ('cst',)], writes=[('RN', ph, ri)])
                        self.rcp(RN[ri], RN[ri], reads=[('RN', ph, ri)], writes=[('RN', ph, ri)])
                        self.tt('dve', CV[:, x3, tsl], CV[:, x3, tsl], RN[ri], ALU.mult,
                                reads=[('CV', ph, x3, tt), ('RN', ph, ri)], writes=[('CV', ph, x3, tt)])
            self.mset('dve', S32, 0.0, writes=[('S32', ph)])
            self.mset('dve', Sb, 0.0, writes=[('Sb', ph)])
            gain = lp[:, 16 + 512 + h * 128:16 + 512 + (h + 1) * 128].unsqueeze(1)
            def chunk_gen(c, T, si):
                csl = slice(c * 128, (c + 1) * 128)
                tq = c // 4
                qn, kn, vt = CV[:, 0, csl], CV[:, 1, csl], CV[:, 2, csl]
                qk_keys = [('CV', ph, 0, tq), ('CV', ph, 1, tq)]
                kk = lambda nm: (nm, ph, si)
                bt = 7
                pt = self.pbank[bt][:].bitcast(BF16)[:, 0:256]
                self.tr(pt[:, 0:128], kn, self.ident_b[:], reads=[('CV', ph, 1, tq), ('ident_b',)], writes=[('pb', bt)])
                self.tr(pt[:, 128:256], vt, self.ident_b[:], reads=[('CV', ph, 2, tq), ('ident_b',)], writes=[('pb', bt)])
                self.ts('dve', T['rk'], pt[:, 0:128], BEG[:, c, h:h + 1], ALU.mult, reads=[('pb', bt), ('BEG', ph)], writes=[kk('rk')])
                self.ts('dve', T['kh'], pt[:, 0:128], KH[:, c, h:h + 1], ALU.mult, reads=[('pb', bt), ('KH', ph)], writes=[kk('kh')])
                self.ts('dve', T['bv'], pt[:, 128:256], BETA[:, c, h:h + 1], ALU.mult, reads=[('pb', bt), ('BETA', ph)], writes=[kk('bv')])
                r = T['rt']
                self.ts('dve', r, self.cst(C_TRIU), LA[:, c, h:h + 1], ALU.mult, reads=[('LA', ph), ('cst',)], writes=[kk('rt')])
                yield
                bT, bS = 4, 6
                pT, pS = self.pbank[bT][:, 0:128], self.pbank[bS][:, 0:128]
                self.mm(pT, self.cst(C_MGT), r, start=True, stop=False, reads=[kk('rt'), ('cst',)], writes=[('pb', bT)])
                self.mm(pT, self.ident_f, self.cst(C_NEGT), start=False, stop=True, reads=[('cst',)], writes=[('pb', bT)])
                self.mm(pS, r, self.cst(C_MGT), start=True, stop=False, reads=[kk('rt'), ('cst',)], writes=[('pb', bS)])
                self.mm(pS, self.ident_f, self.cst(C_NEGS), start=False, stop=True, reads=[('cst',)], writes=[('pb', bS)])
                self.act(T['dT'], pT, AF.Exp, reads=[('pb', bT)], writes=[kk('dT')])
                self.act(T['dS'], pS, AF.Exp, reads=[('pb', bS)], writes=[kk('dS')])
                yield
                bkk = self.rr('pbG', 2)
                pkk = self.pbank[bkk][:, 0:128]
                pqk = self.pbank[bkk][:, 128:256]
                self.mm(pkk, kn, kn, reads=[('CV', ph, 1, tq)], writes=[('pb', bkk)])
                self.mm(pqk, kn, qn, reads=qk_keys, writes=[('pb', bkk)])
                self.stt('dve', T['N32'], pkk, BETA[:, c, h:h + 1], T['dS'], ALU.mult, ALU.mult,
                         reads=[('pb', bkk), ('BETA', ph), kk('dS')], writes=[kk('N32')])
                self.stt('dve', T['QKT'], pqk, 128.0 ** -0.5, T['dT'], ALU.mult, ALU.mult, reads=[('pb', bkk), kk('dT')],
                         writes=[kk('QKT')])
                yield
                bm = 2 + self.rr('pbO', 2)
                pm = self.pbank[bm][:, 0:128]
                self.tr(pm, T['N32'], self.ident_f, reads=[kk('N32'), ('cst',)], writes=[('pb', bm)])
                m3i = self.rr('sh_m32', 2)
                M32, m32k = SH['M32'][m3i], ('M32', ph, m3i)
                self.cp('act', M32, pm, reads=[('pb', bm)], writes=[m32k])
                Nb, Mb, nbk, mbk = T['NbA'], T['MbA'], kk('NbA'), kk('MbA')
                Nb2, Mb2, nbk2, mbk2 = T['NbB'], T['MbB'], kk('NbB'), kk('MbB')
                self.tt('pool', Nb, T['N32'], MSK[:, 0, :], ALU.mult, reads=[kk('N32'), ('MSK', ph)], writes=[nbk])
                self.tt('pool', Mb, M32, MSK[:, 0, :], ALU.mult, reads=[m32k, ('MSK', ph)], writes=[mbk])
                self.tt('pool', T['CL'], T['N32'].unsqueeze(1).to_broadcast([128, 3, 128]), MSK[:, 1:4, :], ALU.mult,
                        reads=[kk('N32'), ('MSK', ph)], writes=[kk('CL')])
                Y, yk, Y2, yk2 = T['YA'], kk('YA'), T['YB'], kk('YB')
                self.tt('dve', Y, self.ident_f, Mb, ALU.subtract, reads=[mbk, ('cst',)], writes=[yk])
                yield
                for step in range(1, 4):
                    bn = self.rr('pbG', 2)
                    pn2 = self.pbank[bn][:, 0:128]
                    bm2 = 2 + self.rr('pbO', 2)
                    pm2 = self.pbank[bm2][:, 0:128]
                    self.mm(pn2, Mb, Nb, reads=[mbk, nbk], writes=[('pb', bn)])
                    if step < 3:
                        self.mm(pm2, Nb, Mb, reads=[mbk, nbk], writes=[('pb', bm2)])
                    self.cp('act', Nb2, pn2, reads=[('pb', bn)], writes=[nbk2])
                    if step < 3:
                        self.cp('dve', Mb2, pm2, reads=[('pb', bm2)], writes=[mbk2])
                        Mb, Mb2, mbk, mbk2 = Mb2, Mb, mbk2, mbk
                    Nb, Nb2, nbk, nbk2 = Nb2, Nb, nbk2, nbk
                    yield
                    bx = (4, 6)[self.rr('pbW', 2)]
                    px2 = self.pbank[bx][:, 0:128]
                    self.mm(px2, Nb, Y, reads=[nbk, yk], writes=[('pb', bx)])
                    self.tt('dve', Y2, px2, Y, ALU.add, reads=[('pb', bx), yk], writes=[yk2])
                    Y, Y2, yk, yk2 = Y2, Y, yk2, yk
                    yield
                for lvl in range(3):
                    bt2 = 2 + self.rr('pbO', 2)
                    pxt = self.pbank[bt2][:, 0:128]
                    ba1 = self.rr('pbG', 2)
                    pa1 = self.pbank[ba1][:, 0:128]
                    self.tr(pxt, Y, self.ident_f, reads=[yk, ('cst',)], writes=[('pb', bt2)])
                    self.mm(pa1, T['CL'][:, lvl, :], Y, reads=[kk('CL'), yk], writes=[('pb', ba1)])
                    XT, A1, xtk, a1k = T['XT'], T['A1'], kk('XT'), kk('A1')
                    self.cp('act', XT, pxt, reads=[('pb', bt2)], writes=[xtk])
                    self.cp('dve', A1, pa1, reads=[('pb', ba1)], writes=[a1k])
                    yield
                    bx = (4, 6)[self.rr('pbW', 2)]
                    px2 = self.pbank[bx][:, 0:128]
                    self.mm(px2, XT, A1, reads=[xtk, a1k], writes=[('pb', bx)])
                    self.tt('dve', Y2, Y, px2, ALU.subtract, reads=[('pb', bx), yk], writes=[yk2])
                    Y, Y2, yk, yk2 = Y2, Y, yk2, yk
                    yield
                X = T['X']
                self.cp('act', X, Y, reads=[yk], writes=[kk('X')])
                yield
                bwk = self.rr('pbG', 2)
                pwk = self.pbank[bwk][:, 0:128]
                self.mm(pwk, T['rk'], X, reads=[kk('rk'), kk('X')], writes=[('pb', bwk)])
                self.act(T['nWk'], pwk, AF.Copy, scale=-1.0, reads=[('pb', bwk)], writes=[kk('nWk')])
                yield
                bu = 2 + self.rr('pbO', 2)
                pu = self.pbank[bu][:, 0:128]
                self.mm(pu, X, T['bv'], start=True, stop=False, reads=[kk('X'), kk('bv')], writes=[('pb', bu)])
                self.mm(pu, T['nWk'], Sb, start=False, stop=True, reads=[kk('nWk'), ('Sb', ph)], writes=[('pb', bu)])
                U = T['U']
                self.cp('act', U, pu, reads=[('pb', bu)], writes=[kk('U')])
                yield
                bo1 = self.rr('pbG', 2)
                po1 = self.pbank[bo1][:, 0:128]
                po2 = self.pbank[bo1][:, 128:256]
                pss = self.pbank[bo1][:, 256:384]
                self.mm(po2, qn, Sb, reads=[('CV', ph, 0, tq), ('Sb', ph)], writes=[('pb', bo1)])
                self.mm(pss, T['kh'], U, reads=[kk('kh'), kk('U')], writes=[('pb', bo1)])
                self.mm(po1, T['QKT'], U, reads=[kk('QKT'), kk('U')], writes=[('pb', bo1)])
                self.stt('dve', S32, S32, DEC[:, c, h:h + 1], pss, ALU.mult, ALU.add,
                         reads=[('S32', ph), ('DEC', ph), ('pb', bo1)], writes=[('S32', ph)])
                self.cp('act', Sb, S32, reads=[('S32', ph)], writes=[('Sb', ph)])
                t2i = self.rr('sh_t2', 2)
                T2, t2k = SH['T2'][t2i], ('T2', ph, t2i)
                self.ts('dve', T2, po2, QSG[:, c, h:h + 1], ALU.mult, reads=[('pb', bo1), ('QSG', ph)], writes=[t2k])
                self.tt('dve', T['NUM'], T2, po1, ALU.add, reads=[t2k, ('pb', bo1)], writes=[kk('NUM')])
                yield
                self.head_post(T['NUM'].unsqueeze(1), [kk('NUM')], 1, 128, gain,
                               ZS[:, c, h * 128:(h + 1) * 128].unsqueeze(1), ('ZS', ph, c),
                               ytok[:, c, h * 128:(h + 1) * 128].unsqueeze(1), ('ZS', ph, c), post)

            self.P.barrier()
            active = []
            nxt = 0
            rounds = 0
            START_EVERY = 4
            while nxt < NCH or active:
                if nxt < NCH and len(active) < KI and rounds % START_EVERY == 0:
                    active.append(chunk_gen(nxt, TS[nxt % KI], nxt % KI))
                    nxt += 1
                for g_ in list(active):
                    try:
                        next(g_)
                    except StopIteration:
                        active.remove(g_)
                rounds += 1
            self.P.barrier()
        self.phase()
        self.view([128, LPW], F32)
        ytok2 = self.view([128, NCH, 512], BF16)
        yT = self.view([128, 4, S], BF16)
        Wo = self.view([128, 4, D], BF16)
        for c in range(NCH):
            self.P.last_w[('ytok', self.ph, c)] = self.P.last_w.get(('ZS', ph, c))
            self.P.last_r[('ytok', self.ph, c)] = {}
        self.emit_outproj(l, 512, 512, ytok2, yT, Wo)


def host_consts(inputs, nlayers):
    f32 = np.float32
    g = np.stack([inputs['ffn1_norm'], inputs['mix_norm'], inputs['ffn2_norm']], axis=1).reshape(3 * NL, D)
    g = np.concatenate([g, inputs['final_norm'][None, :]], axis=0)
    gains = np.ascontiguousarray(g.reshape(3 * NL + 1, NDC, 128).transpose(2, 0, 1)).astype(f32)
    idx = np.arange(128)
    cst = np.zeros((128, CW), f32)
    cst[:, C_ID:C_ID + 128] = np.eye(128)
    cst[:, C_TRIU:C_TRIU + 128] = (idx[:, None] <= idx[None, :])
    cst[:, C_MGT:C_MGT + 128] = (idx[:, None] > idx[None, :])
    cst[:, C_NEGT:C_NEGT + 128] = np.where(idx[None, :] < idx[:, None], NEG, 0.0)
    cst[:, C_NEGS:C_NEGS + 128] = np.where(idx[None, :] >= idx[:, None], NEG, 0.0)
    cst[:, C_ONES:C_ONES + 128] = 1.0
    lg = np.log1p(-np.exp2(-5.0 - np.arange(4, dtype=np.float64)))
    rel = (idx[None, :] - idx[:, None]).astype(np.float64)
    for h in range(4):
        w = np.where(rel >= 0, np.exp(np.where(rel >= 0, rel, 0.0) * lg[h]), 0.0)
        cst[:, C_RW + h * 128:C_RW + (h + 1) * 128] = w
        cst[:, C_RQS + h] = np.exp((idx + 1.0) * lg[h])
        cst[:, C_RKS + h] = np.exp((127.0 - idx) * lg[h]) * 0.125
        cst[:, C_RDEC + h] = np.exp(128.0 * lg[h])
    cst[:, C_EPS] = EPS
    inv_freq = (10000.0 ** (-np.arange(0, 64, 2, dtype=f32) / f32(64))).astype(f32)
    ang = (np.arange(S, dtype=f32)[None, :] * inv_freq[:, None]).astype(f32).astype(np.float64)
    cosr = np.cos(ang)
    sinr = np.sin(ang)
    rope = np.zeros((2, 128, S), f32)
    for p in range(128):
        f = p % 32
        rope[0, p] = cosr[f]
        rope[1, p] = (-sinr[f] if (p % 64) < 32 else sinr[f])
    lp = np.zeros((nlayers, 128, LPW), f32)
    for l in range(nlayers):
        lp[l, :, 0:4] = inputs['m_i_bias'][l][None, :]
        lp[l, :, 4:8] = inputs['m_f_bias'][l][None, :]
        lp[l, :, 8:12] = inputs['g_a_log'][l][None, :]
        lp[l, :, 12:16] = inputs['g_dt_bias'][l][None, :]
        lp[l, :, 16:16 + 256] = inputs['m_out_norm'][l][None, :]
        lp[l, :, 16 + 256:16 + 512] = inputs['r_out_norm'][l][None, :]
        lp[l, :, 16 + 512:16 + 1024] = inputs['g_out_norm'][l][None, :]
        cw = inputs['g_conv'][l]
        for x3 in range(3):
            for h in range(4):
                ch = x3 * 512 + h * 128 + idx
                lp[l, :, 16 + 1024 + (x3 * 4 + h) * 4:16 + 1024 + (x3 * 4 + h) * 4 + 4] = cw[:, ch].T
    msk = np.zeros((128, 4, 128), f32)
    blk = lambda b: (idx[:, None] // b == idx[None, :] // b).astype(f32)
    msk[:, 0] = blk(16)
    msk[:, 1] = blk(32) - blk(16)
    msk[:, 2] = blk(64) - blk(32)
    msk[:, 3] = blk(128) - blk(64)
    return {'gains': gains, 'cst': cst, 'rope': rope, 'lp': lp, 'msk': msk.reshape(128, 512)}


_CACHE = {}


def run(inputs, nseq_per_core=4, nlayers=NL, stages=('ffn1', 'mix', 'ffn2'), ncores=8, trace=False):
    key = (nseq_per_core, nlayers, tuple(stages))
    if key not in _CACHE:
        _CACHE[key] = Builder(nseq_per_core, nlayers, stages).build()
    nc = _CACHE[key]
    consts = host_consts(inputs, nlayers)
    x = np.ascontiguousarray(inputs['x'], dtype=np.float32)
    in_maps = []
    shared = {}
    for f in (1, 2):
        for w in ('w_gate', 'w_up', 'w_down'):
            shared[f'ffn{f}_{w}'] = np.ascontiguousarray(inputs[f'ffn{f}_{w}'][:nlayers], dtype=np.float32)
    shared['w_in'] = np.ascontiguousarray(inputs['w_in'][:nlayers], dtype=np.float32)
    shared['w_out'] = np.ascontiguousarray(inputs['w_out'][:nlayers], dtype=np.float32)
    for c in range(ncores):
        m = {'x': x[c * nseq_per_core:(c + 1) * nseq_per_core]}
        m.update(shared)
        m.update(consts)
        in_maps.append(m)
    res = run_bass_kernel_spmd(nc, in_maps, core_ids=list(range(ncores)), trace=trace)
    out = np.concatenate([r['out'] for r in res.results], axis=0)
    return out, res


def kernel(**inputs):
    out, _ = run(inputs)
    return out.astype(np.float32)
```

```python
import itertools
import math
import os
from contextlib import ExitStack

import numpy as np
import concourse.bass as bass
import concourse.mybir as mybir
from concourse.bass_utils import run_bass_kernel_spmd

F32 = mybir.dt.float32
BF16 = mybir.dt.bfloat16
AF = mybir.ActivationFunctionType
ALU = mybir.AluOpType
AX = mybir.AxisListType

D = 1024
S = 2048
DFF = 2816
NL = 4
NFC = DFF // 128
NDC = D // 128
NCH = S // 128
INW = 4112
EPS = 1e-6
SEM_BLOCK = 16000
NEG = -30000.0
LPW = 16 + 1024 + 48
ARENA_BYTES = 104 * 1024

C_ID, C_TRIU, C_MGT, C_NEGT, C_NEGS, C_ONES, C_RW, C_RQS, C_RKS, C_RDEC, C_EPS = \
    0, 128, 256, 384, 512, 640, 768, 1280, 1284, 1288, 1292
CW = 1296

ENG = ('pe', 'act', 'dve', 'pool', 'sp')


def K(name, *ranges):
    rs = [r if isinstance(r, (list, tuple, range)) else [r] for r in ranges]
    return [(name,) + t for t in itertools.product(*rs)]


class Slot:
    def __init__(self, name):
        self.name = name
        self.count = 0
        self.sem = None


class Op:
    __slots__ = ('eng', 'fn', 'idx', 'waits', 'signal', 'count', 'slot', 'slot_count', 'snap', 'dsnap')

    def __init__(self, eng, fn, idx):
        self.eng = eng
        self.fn = fn
        self.idx = idx
        self.waits = []
        self.signal = False
        self.count = None
        self.slot = None
        self.slot_count = 0
        self.snap = None
        self.dsnap = None


class Prog:
    def __init__(self):
        self.ops = {e: [] for e in ENG}
        self.last_w = {}
        self.last_r = {}
        self.seen = {e: {f: -1 for f in ENG} for e in ENG}
        self.dseen = {e: {} for e in ENG}
        self.slots = []

    def slot(self, name):
        s = Slot(name)
        self.slots.append(s)
        return s

    def _wait_on(self, op, eng, d):
        seen = self.seen[eng]
        dseen = self.dseen[eng]
        if d.slot is not None:
            if dseen.get(d.slot, 0) < d.slot_count:
                dseen[d.slot] = d.slot_count
                op.waits.append(('dma', d.slot, d.slot_count))
                for f, v in d.snap.items():
                    if v > seen[f]:
                        seen[f] = v
        else:
            if d.idx > seen[d.eng]:
                d.signal = True
                op.waits.append(('op', d))
                seen[d.eng] = d.idx
                for g, v in d.snap.items():
                    if v > seen[g]:
                        seen[g] = v
                for s_, v in d.dsnap.items():
                    if dseen.get(s_, 0) < v:
                        dseen[s_] = v

    def emit(self, eng, fn, reads=(), writes=(), slot=None):
        op = Op(eng, fn, len(self.ops[eng]))
        is_dma = slot is not None
        pbr = [k for k in reads if k[0] == 'pb']
        if pbr:
            writes = list(writes) + [k for k in pbr if k not in writes]
        deps = []
        for k in reads:
            w = self.last_w.get(k)
            if w is not None:
                deps.append(w)
        for k in writes:
            w = self.last_w.get(k)
            if w is not None and (w.eng != eng or w.slot is not None or is_dma):
                deps.append(w)
            rd = self.last_r.get(k)
            if rd:
                for r in rd.values():
                    if r.eng != eng or r.slot is not None or is_dma:
                        deps.append(r)
        best = {}
        for d in deps:
            if d.slot is not None:
                self._wait_on(op, eng, d)
            elif d.eng not in best or d.idx > best[d.eng].idx:
                best[d.eng] = d
        for d in best.values():
            self._wait_on(op, eng, d)
        if is_dma:
            slot.count += 1
            op.slot = slot
            op.slot_count = slot.count
        op.snap = dict(self.seen[eng])
        op.dsnap = dict(self.dseen[eng])
        if not is_dma and op.snap[eng] < op.idx - 1:
            op.snap[eng] = op.idx - 1
        self.ops[eng].append(op)
        for k in reads:
            self.last_r.setdefault(k, {})[eng if not is_dma else ('dma', id(op))] = op
        for k in writes:
            self.last_w[k] = op
            self.last_r[k] = {}
        return op

    def barrier(self):
        lasts = {}
        for e in ENG:
            for op in reversed(self.ops[e]):
                if op.fn is not None and op.slot is None:
                    lasts[e] = op
                    break
        dmas = []
        for e in ENG:
            latest = {}
            for op in self.ops[e]:
                if op.slot is not None:
                    latest[op.slot] = op
            dmas.extend(latest.values())
        for e in ENG:
            op = Op(e, None, len(self.ops[e]))
            for f, d in lasts.items():
                if f != e:
                    self._wait_on(op, e, d)
            for d in dmas:
                self._wait_on(op, e, d)
            op.snap = dict(self.seen[e])
            op.dsnap = dict(self.dseen[e])
            self.ops[e].append(op)

    def finalize(self, nc, es, block):
        self.sems = {}
        for e in ENG:
            c = 0
            for op in self.ops[e]:
                if op.signal:
                    op.count = c
                    c += 1
            nblk = (c + SEM_BLOCK - 1) // SEM_BLOCK
            self.sems[e] = [es.enter_context(nc.semaphore(f"s_{e}_{i}")) for i in range(max(nblk, 1))]
        for s in self.slots:
            if s.count > 0:
                s.sem = es.enter_context(nc.semaphore(f"d_{s.name}"))

        def run(e):
            def body(h):
                for op in self.ops[e]:
                    for w in op.waits:
                        if w[0] == 'dma':
                            h.wait_ge(w[1].sem, 16 * w[2])
                        else:
                            d = w[1]
                            h.wait_ge(self.sems[d.eng][d.count // SEM_BLOCK], d.count % SEM_BLOCK + 1)
                    if op.fn is None:
                        continue
                    ins = op.fn(h)
                    if op.slot is not None:
                        ins.then_inc(op.slot.sem, 16)
                    elif op.signal:
                        ins.then_inc(self.sems[e][op.count // SEM_BLOCK], 1)
            return body

        block.tensor(run('pe'))
        block.scalar(run('act'))
        block.vector(run('dve'))
        block.gpsimd(run('pool'))
        block.sync(run('sp'))


class Builder:
    def __init__(self, nseq, nlayers, stages):
        self.nseq = nseq
        self.nlayers = nlayers
        self.stages = stages
        self.P = Prog()
        self.nc = bass.Bass("TRN2", target_bir_lowering=False)
        self.rot = {}
        self.out_keys = []
        self.slots = {}
        self.ph = 0
        self.apos = 0

    def rr(self, name, n):
        v = self.rot.get(name, 0)
        self.rot[name] = v + 1
        return v % n

    def slot(self, name):
        if name not in self.slots:
            self.slots[name] = self.P.slot(name)
        return self.slots[name]

    def phase(self):
        self.P.barrier()
        self.ph += 1
        self.apos = 0

    def view(self, shape, dt):
        n = 1
        for s_ in shape[1:]:
            n *= s_
        nbytes = n * (4 if dt == F32 else 2)
        nbytes = (nbytes + 63) // 64 * 64
        assert self.apos + nbytes <= ARENA_BYTES, (self.apos, nbytes, shape)
        c0 = self.apos // 4
        ap = self.arena[:, c0:c0 + nbytes // 4]
        self.apos += nbytes
        if dt != F32:
            ap = ap.bitcast(dt)
        ap = ap[:, 0:n]
        if len(shape) == 3:
            ap = ap.rearrange("p (a b) -> p a b", a=shape[1])
        elif len(shape) == 4:
            ap = ap.rearrange("p (a b c) -> p a b c", a=shape[1], b=shape[2])
        return ap

    def mm(self, out, lhsT, rhs, start=True, stop=True, reads=(), writes=()):
        self.P.emit('pe', lambda h: h.matmul(out, lhsT, rhs, start=start, stop=stop), reads, writes)

    def tr(self, out, in_, ident, reads=(), writes=()):
        self.P.emit('pe', lambda h: h.transpose(out, in_, ident), reads, writes)

    def act(self, out, in_, func, reads=(), writes=(), **kw):
        self.P.emit('act', lambda h: h.activation(out=out, in_=in_, func=func, **kw), reads, writes)

    def tt(self, eng, out, in0, in1, op, reads=(), writes=()):
        self.P.emit(eng, lambda h: h.tensor_tensor(out=out, in0=in0, in1=in1, op=op), reads, writes)

    def ts(self, eng, out, in0, s1, op0, s2=None, op1=None, reads=(), writes=()):
        if op1 is None:
            self.P.emit(eng, lambda h: h.tensor_scalar(out=out, in0=in0, scalar1=s1, scalar2=None, op0=op0), reads, writes)
        else:
            self.P.emit(eng, lambda h: h.tensor_scalar(out=out, in0=in0, scalar1=s1, scalar2=s2, op0=op0, op1=op1), reads, writes)

    def stt(self, eng, out, in0, scalar, in1, op0, op1, reads=(), writes=()):
        self.P.emit(eng, lambda h: h.scalar_tensor_tensor(out=out, in0=in0, scalar=scalar, in1=in1, op0=op0, op1=op1), reads, writes)

    def cp(self, eng, out, in_, reads=(), writes=()):
        if eng == 'act':
            self.P.emit('act', lambda h: h.activation(out=out, in_=in_, func=AF.Copy), reads, writes)
        else:
            self.P.emit(eng, lambda h: h.tensor_copy(out=out, in_=in_), reads, writes)

    def rcp(self, out, in_, reads=(), writes=()):
        self.P.emit('dve', lambda h: h.reciprocal(out=out, in_=in_), reads, writes)

    def mset(self, eng, ap, val, writes=()):
        self.P.emit(eng, lambda h: h.memset(ap, val), (), writes)

    def dma(self, eng, out, in_, slotname, reads=(), writes=()):
        self.P.emit(eng, lambda h: h.dma_start(out=out, in_=in_), reads, writes, slot=self.slot(slotname))

    def cst(self, c0, w=128):
        return self.cstt[:, c0:c0 + w]

    def build(self):
        nc = self.nc
        P = self.P
        dr = {}

        def din(name, shape, dt=F32):
            dr[name] = nc.dram_tensor(name, list(shape), dt, kind="ExternalInput").ap()

        nl = self.nlayers
        din('x', [self.nseq, S, D])
        for f in (1, 2):
            din(f'ffn{f}_w_gate', [nl, D, DFF])
            din(f'ffn{f}_w_up', [nl, D, DFF])
            din(f'ffn{f}_w_down', [nl, DFF, D])
        din('w_in', [nl, D, INW])
        din('w_out', [nl, D, D])
        din('gains', [128, 3 * NL + 1, NDC])
        din('cst', [128, CW])
        din('lp', [nl, 128, LPW])
        din('rope', [2, 128, S])
        din('msk', [128, 512])
        self.dr = dr
        self.out = nc.dram_tensor('out', [self.nseq, S, D], F32, kind="ExternalOutput").ap()

        with ExitStack() as es:
            self.es = es

            def sb(name, shape, dt):
                return es.enter_context(nc.sbuf_tensor('sb_' + name, list(shape), dt))

            self.hT = sb('hT', [128, NDC, S], F32)
            self.aT = sb('aT', [128, NDC, S], BF16)
            self.gains = sb('gains', [128, 3 * NL + 1, NDC], F32)
            self.cstt = sb('cst', [128, CW], F32)
            self.ident_b = sb('ident_b', [128, 128], BF16)
            self.onesM = sb('onesM', [128, 128], BF16)
            self.ones_b = sb('ones_b', [128, 128], BF16)
            self.arena = sb('arena', [128, ARENA_BYTES // 4], F32)
            self.ident_f = self.cst(C_ID)
            self.epsc = self.cst(C_EPS, 1)
            self.onec = self.cst(C_ONES, 1)
            self.pbank = [es.enter_context(nc.psum_tensor(f'ps_b{i}', [128, 512], F32)) for i in range(8)]
            block = es.enter_context(nc.Block())

            self.emit_consts()
            for s in range(self.nseq):
                self.emit_load_x(s)
                for l in range(self.nlayers):
                    if 'ffn1' in self.stages:
                        self.emit_ffn(l, 1)
                    if any(st.startswith('mix') for st in self.stages):
                        self.emit_mixer(l)
                    if 'ffn2' in self.stages:
                        self.emit_ffn(l, 2)
                self.emit_final(s)
            P.emit('sp', None, reads=self.out_keys)
            P.finalize(nc, es, block)
        return nc

    def emit_consts(self):
        dr = self.dr
        self.dma('sp', self.gains[:], dr['gains'][:, :, :], 'gains', writes=[('gains',)])
        self.dma('sp', self.cstt[:], dr['cst'][:, :], 'cst', writes=[('cst',)])
        self.cp('dve', self.ident_b[:], self.ident_f, reads=[('cst',)], writes=[('ident_b',)])
        self.mset('dve', self.onesM[:], 1.0 / D, writes=[('onesM',)])
        self.mset('dve', self.ones_b[:], 1.0, writes=[('ones_b',)])

    def emit_load_x(self, s):
        self.phase()
        x = self.dr['x']
        xin = [self.view([128, D], F32) for _ in range(2)]
        for t in range(NCH):
            i = self.rr('xin', 2)
            self.dma('sp', xin[i], x[s, t * 128:(t + 1) * 128, :], f'xin{i}',
                     writes=[('xin', self.ph, i, 0), ('xin', self.ph, i, 1)])
            for half in range(2):
                b = 6 + self.rr('pbT', 2)
                pb = self.pbank[b]
                for q in range(4):
                    dc = half * 4 + q
                    self.tr(pb[:, q * 128:(q + 1) * 128], xin[i][:, dc * 128:(dc + 1) * 128], self.ident_f,
                            reads=[('xin', self.ph, i, half), ('cst',)], writes=[('pb', b)])
                outap = self.hT[:, half * 4:(half + 1) * 4, t * 128:(t + 1) * 128]
                inap = pb[:, :].rearrange("p (q t) -> p q t", q=4)
                self.cp('act' if half == 0 else 'dve', outap, inap, reads=[('pb', b)],
                        writes=K('h', range(half * 4, half * 4 + 4), t))

    def emit_norm(self, tt, out_fn):
        i = self.rr('sq', len(self.sq))
        sq = self.sq[i]
        rstd = self.rstd[i]
        tsl = slice(tt * 512, (tt + 1) * 512)
        hkeys = K('h', range(NDC), range(tt * 4, tt * 4 + 4))
        self.act(sq, self.hT[:, :, tsl], AF.Square, reads=hkeys, writes=[('sq', self.ph, i)])
        pb = self.pbank[5]
        for dc in range(NDC):
            self.mm(pb[:], self.onesM[:], sq[:, dc, :], start=(dc == 0), stop=(dc == NDC - 1),
                    reads=[('sq', self.ph, i), ('onesM',)], writes=[('pb', 5)])
        self.act(rstd, pb[:], AF.Sqrt, bias=self.epsc, reads=[('pb', 5), ('cst',)], writes=[('rstd', self.ph, i)])
        self.rcp(rstd, rstd, reads=[('rstd', self.ph, i)], writes=[('rstd', self.ph, i)])
        for dc in range(NDC):
            out_fn(dc, rstd, ('rstd', self.ph, i), tsl, tt)

    def alloc_norm(self, n=2):
        self.sq = [self.view([128, NDC, 512], BF16) for _ in range(n)]
        self.rstd = [self.view([128, 512], F32) for _ in range(n)]

    def emit_norm_to_aT(self, gidx):
        for tt in range(4):
            def out_fn(dc, rstd, rkey, tsl, tt):
                self.stt('dve', self.aT[:, dc, tsl], self.hT[:, dc, tsl], self.gains[:, gidx, dc:dc + 1], rstd,
                         ALU.mult, ALU.mult,
                         reads=K('h', dc, range(tt * 4, tt * 4 + 4)) + [rkey, ('gains',)],
                         writes=K('a', dc, range(tt * 4, tt * 4 + 4)))
            self.emit_norm(tt, out_fn)

    def emit_final(self, s):
        self.phase()
        self.alloc_norm(2)
        xo_ = [self.view([128, D], F32) for _ in range(2)]
        gidx = 3 * NL
        for tt in range(4):
            def out_fn(dc, rstd, rkey, tsl, tt):
                hk = K('h', dc, range(tt * 4, tt * 4 + 4))
                self.stt('dve', self.hT[:, dc, tsl], self.hT[:, dc, tsl], self.gains[:, gidx, dc:dc + 1], rstd,
                         ALU.mult, ALU.mult, reads=hk + [rkey, ('gains',)], writes=hk)
            self.emit_norm(tt, out_fn)
        for t in range(NCH):
            i = self.rr('xo', 2)
            xo = xo_[i]
            for half in range(2):
                b = 6 + self.rr('pbT', 2)
                pb = self.pbank[b]
                for q in range(4):
                    dc = half * 4 + q
                    self.tr(pb[:, q * 128:(q + 1) * 128], self.hT[:, dc, t * 128:(t + 1) * 128], self.ident_f,
                            reads=[('h', dc, t), ('cst',)], writes=[('pb', b)])
                self.cp('act' if half == 0 else 'dve', xo[:, half * 512:(half + 1) * 512], pb[:],
                        reads=[('pb', b)], writes=[('xo', self.ph, i, half)])
            self.dma('sp', self.out[s, t * 128:(t + 1) * 128, :], xo, f'xout{i}',
                     reads=[('xo', self.ph, i, 0), ('xo', self.ph, i, 1)], writes=[('out_dram', s, t)])
            self.out_keys.append(('out_dram', s, t))

    def emit_ffn(self, l, which):
        self.phase()
        ph = self.ph
        dr = self.dr
        GF = 4
        self.alloc_norm(2)
        wg_ = [self.view([128, NDC, GF * 128], BF16) for _ in range(2)]
        wu_ = [self.view([128, NDC, GF * 128], BF16) for _ in range(2)]
        wd_ = [self.view([128, GF, D], BF16) for _ in range(2)]
        hid_ = [self.view([128, GF, S], BF16) for _ in range(2)]
        sg_ = [self.view([128, 512], BF16) for _ in range(2)]
        self.emit_norm_to_aT(3 * l + (0 if which == 1 else 2))
        Wg = dr[f'ffn{which}_w_gate'][l].rearrange("(dc p) f -> p dc f", p=128)
        Wu = dr[f'ffn{which}_w_up'][l].rearrange("(dc p) f -> p dc f", p=128)
        Wd = dr[f'ffn{which}_w_down'][l].rearrange("(fc p) d -> p fc d", p=128)
        groups = [(g0, min(GF, NFC - g0)) for g0 in range(0, NFC, GF)]
        for (fc0, nfc) in groups:
            ws = self.rr('wslot', 2)
            wg, wu, wd = wg_[ws], wu_[ws], wd_[ws]
            self.dma('pool', wg[:, :, 0:nfc * 128], Wg[:, :, fc0 * 128:(fc0 + nfc) * 128], f'wg{ws}', writes=[('wg', ph, ws)])
            self.dma('pool', wu[:, :, 0:nfc * 128], Wu[:, :, fc0 * 128:(fc0 + nfc) * 128], f'wu{ws}', writes=[('wu', ph, ws)])
            self.dma('pool', wd[:, 0:nfc, :], Wd[:, fc0:fc0 + nfc, :], f'wd{ws}', writes=[('wd', ph, ws)])
            hs = self.rr('hslot', 2)
            hid = hid_[hs]
            for fci in range(nfc):
                for tt in range(4):
                    tsl = slice(tt * 512, (tt + 1) * 512)
                    akeys = K('a', range(NDC), range(tt * 4, tt * 4 + 4))
                    bi = self.rr('pbG', 2)
                    pg, pu = self.pbank[bi], self.pbank[2 + bi]
                    for dc in range(NDC):
                        self.mm(pg[:], wg[:, dc, fci * 128:(fci + 1) * 128], self.aT[:, dc, tsl],
                                start=(dc == 0), stop=(dc == NDC - 1),
                                reads=[('wg', ph, ws)] + akeys, writes=[('pb', bi)])
                    for dc in range(NDC):
                        self.mm(pu[:], wu[:, dc, fci * 128:(fci + 1) * 128], self.aT[:, dc, tsl],
                                start=(dc == 0), stop=(dc == NDC - 1),
                                reads=[('wu', ph, ws)] + akeys, writes=[('pb', 2 + bi)])
                    si = self.rr('sg', 2)
                    sg = sg_[si]
                    self.act(sg, pg[:], AF.Silu, reads=[('pb', bi)], writes=[('sg', ph, si)])
                    self.tt('dve', hid[:, fci, tsl], pu[:], sg, ALU.mult,
                            reads=[('pb', 2 + bi), ('sg', ph, si)], writes=[('hid', ph, hs, fci, tt)])
            for dc in range(NDC):
                for tt in range(4):
                    tsl = slice(tt * 512, (tt + 1) * 512)
                    bi = (4, 6, 7)[self.rr('pbD', 3)]
                    pd = self.pbank[bi]
                    for fci in range(nfc):
                        self.mm(pd[:], wd[:, fci, dc * 128:(dc + 1) * 128], hid[:, fci, tsl],
                                start=(fci == 0), stop=(fci == nfc - 1),
                                reads=[('wd', ph, ws), ('hid', ph, hs, fci, tt)], writes=[('pb', bi)])
                    hk = K('h', dc, range(tt * 4, tt * 4 + 4))
                    self.stt('dve', self.hT[:, dc, tsl], pd[:], 0.5, self.hT[:, dc, tsl], ALU.mult, ALU.add,
                             reads=[('pb', bi)] + hk, writes=hk)

    def emit_mixer(self, l):
        self.phase()
        self.alloc_norm(2)
        self.emit_norm_to_aT(3 * l + 1)
        if 'mix' in self.stages or 'mixm' in self.stages:
            self.emit_lin_group(l, 'm')
        if 'mix' in self.stages or 'mixr' in self.stages:
            self.emit_lin_group(l, 'r')
        if 'mix' in self.stages or 'mixg' in self.stages:
            self.emit_gdn(l)

    def load_lp(self, l):
        lp = self.view([128, LPW], F32)
        self.dma('sp', lp, self.dr['lp'][l], 'lp', writes=[('lp', self.ph)])
        return lp

    def gate_cumsum(self, LF, lfkey, CG, cgkey):
        pb = self.pbank[5]
        pv = pb[:, 0:NCH * 8].rearrange("p (c e) -> p c e", c=NCH)
        for c in range(NCH):
            self.mm(pv[:, c, 0:4], self.cst(C_TRIU), LF[:, c, :], reads=[lfkey, ('cst',)], writes=[('pb', 5)])
            self.mm(pv[:, c, 4:8], self.cst(C_ONES), LF[:, c, :], reads=[lfkey, ('cst',)], writes=[('pb', 5)])
        self.cp('act', CG, pv, reads=[('pb', 5)], writes=[cgkey])

    def head_post(self, num, numkeys, nh, dh, gain, gate_c, gatekey, ytok_c, ykey, scr):
        ph = self.ph
        SQ, SS, RS, T1 = scr
        i = self.rr('hp', 2)
        sqv = SQ[i][:, 0:nh * dh].rearrange("p (h e) -> p h e", h=nh)
        t1v = T1[i][:, 0:nh * dh].rearrange("p (h e) -> p h e", h=nh)
        ss = SS[i][:, 0:nh]
        rs = RS[i][:, 0:nh]
        self.tt('pool', sqv, num, num, ALU.mult, reads=numkeys, writes=[('hpsq', ph, i)])
        self.P.emit('dve', lambda h: h.reduce_sum(out=ss, in_=sqv, axis=AX.X),
                    reads=[('hpsq', ph, i)], writes=[('hpss', ph, i)])
        self.act(rs, ss, AF.Ln, bias=self.epsc, scale=1.0 / dh, reads=[('hpss', ph, i), ('cst',)], writes=[('hprs', ph, i)])
        self.act(rs, rs, AF.Exp, scale=-0.5, reads=[('hprs', ph, i)], writes=[('hprs', ph, i)])
        self.tt('dve', t1v, num, rs.unsqueeze(2).to_broadcast([128, nh, dh]), ALU.mult,
                reads=numkeys + [('hprs', ph, i)], writes=[('hpt1', ph, i)])
        self.tt('pool', t1v, t1v, gain, ALU.mult, reads=[('hpt1', ph, i), ('lp', ph)], writes=[('hpt1', ph, i)])
        self.tt('dve', ytok_c, t1v, gate_c, ALU.mult, reads=[('hpt1', ph, i), gatekey], writes=[ykey])

    def alloc_post(self, width):
        SQ = [self.view([128, width], F32) for _ in range(2)]
        SS = [self.view([128, 4], F32) for _ in range(2)]
        RS = [self.view([128, 4], F32) for _ in range(2)]
        T1 = [self.view([128, width], F32) for _ in range(2)]
        return (SQ, SS, RS, T1)

    def emit_outproj(self, l, m0, width, ytok, yT, Wo):
        ph = self.ph
        nmc = width // 128
        Wod = self.dr['w_out'][l].rearrange("(mc p) d -> p mc d", p=128)
        self.dma('pool', Wo[:, 0:nmc, :], Wod[:, m0 // 128:m0 // 128 + nmc, :], 'wo', writes=[('wo', ph)])
        for c in range(NCH):
            b = 6 + self.rr('pbT', 2)
            pbv = self.pbank[b][:].bitcast(BF16)[:, 0:nmc * 128].rearrange("p (m t) -> p m t", m=nmc)
            for mc in range(nmc):
                self.tr(pbv[:, mc, :], ytok[:, c, mc * 128:(mc + 1) * 128], self.ident_b[:],
                        reads=[('ytok', ph, c), ('ident_b',)], writes=[('pb', b)])
            self.cp('act' if c % 2 == 0 else 'dve', yT[:, 0:nmc, c * 128:(c + 1) * 128], pbv,
                    reads=[('pb', b)], writes=[('yT', ph, c)])
        for dc in range(NDC):
            for tt in range(4):
                tsl = slice(tt * 512, (tt + 1) * 512)
                bi = (4, 6, 7)[self.rr('pbD', 3)]
                pd = self.pbank[bi]
                for mc in range(nmc):
                    self.mm(pd[:], Wo[:, mc, dc * 128:(dc + 1) * 128], yT[:, mc, tsl],
                            start=(mc == 0), stop=(mc == nmc - 1),
                            reads=[('wo', ph)] + K('yT', ph, range(tt * 4, tt * 4 + 4)), writes=[('pb', bi)])
                hk = K('h', dc, range(tt * 4, tt * 4 + 4))
                self.tt('dve', self.hT[:, dc, tsl], pd[:], self.hT[:, dc, tsl], ALU.add,
                        reads=[('pb', bi)] + hk, writes=hk)

    def decay_mats(self, LA, lakey, c, h, want_T, want_S, pT, pS, bT, bS, rt):
        ph = self.ph
        ri = self.rr('rt', len(rt))
        r = rt[ri]
        self.ts('dve', r, self.cst(C_TRIU), LA[:, c, h:h + 1], ALU.mult, reads=[lakey, ('cst',)], writes=[('rt', ph, ri)])
        if want_T:
            self.mm(pT, self.cst(C_MGT), r, start=True, stop=False, reads=[('rt', ph, ri), ('cst',)], writes=[('pb', bT)])
            self.mm(pT, self.ident_f, self.cst(C_NEGT), start=False, stop=True, reads=[('cst',)], writes=[('pb', bT)])
        if want_S:
            self.mm(pS, r, self.cst(C_MGT), start=True, stop=False, reads=[('rt', ph, ri), ('cst',)], writes=[('pb', bS)])
            self.mm(pS, self.ident_f, self.cst(C_NEGS), start=False, stop=True, reads=[('cst',)], writes=[('pb', bS)])

    def emit_lin_group(self, l, kind):
        self.phase()
        ph = self.ph
        dr = self.dr
        is_m = kind == 'm'
        col0 = 0 if is_m else 1032
        m0 = 0 if is_m else 256
        E = 65 if is_m else 64
        lp = self.load_lp(l)
        W = self.view([128, NDC, 1536], BF16)
        qkT = self.view([128, 6, S], BF16)
        vtok = self.view([128, NCH, 4, E], BF16)
        gate = self.view([128, NCH, 256], BF16)
        ytok = gate
        G = self.view([128, NCH, 8], F32)
        CG = self.view([128, NCH, 8], F32)
        LF = self.view([128, NCH, 4], F32)
        QS = self.view([128, NCH, 4], F32)
        KS2 = self.view([128, NCH, 4], F32)
        DEC = self.view([128, NCH, 4], F32)
        TMP = self.view([128, NCH, 4], F32)
        C32 = self.view([128, 2, E], F32)
        Cb = self.view([128, 2, E], BF16)
        KL = 3
        wT_ = [self.view([128, 4, 128], F32) for _ in range(KL)] if is_m else None
        rt4_ = [[self.view([128, 128], F32) for _ in range(4)] for _ in range(KL)] if is_m else None
        scm_ = [self.view([128, 4, 128], BF16) for _ in range(KL)]
        khat_ = [self.view([128, 4, 64], BF16) for _ in range(KL)]
        T2_ = [self.view([128, 4, E], F32) for _ in range(KL)]
        NUM_ = [self.view([128, 4, E], F32) for _ in range(KL)]
        HN_ = [self.view([128, 4, 64], F32) for _ in range(KL)] if is_m else None
        R_ = [self.view([128, 4], F32) for _ in range(KL)]
        rt = [self.view([128, 128], F32) for _ in range(2)]
        post = self.alloc_post(256)
        Wd = dr['w_in'][l].rearrange("(dc p) f -> p dc f", p=128)
        self.dma('pool', W[:, :, 0:(1032 if is_m else 1024)], Wd[:, :, col0:col0 + (1032 if is_m else 1024)], 'wgrp',
                 writes=[('W', ph)])
        if not is_m:
            ROPE = [self.view([128, S], BF16) for _ in range(2)]
            rtmp = [self.view([128, 512], F32) for _ in range(2)]
            rtmpB = [self.view([128, 512], F32) for _ in range(2)]
            for which in range(2):
                for two in range(2):
                    src = W[:, :, which * 256:(which + 1) * 256].rearrange(
                        "p dc (h two f) -> p dc h two f", h=4, two=2)[:, :, :, two, :]
                    dst = W[:, :, 1024 + which * 256:1024 + (which + 1) * 256].rearrange(
                        "p dc (h two f) -> p dc h two f", h=4, two=2)[:, :, :, 1 - two, :]
                    self.cp('act' if two == 0 else 'dve', dst, src, reads=[('W', ph)], writes=[('Wsw', ph)])
            self.dma('pool', ROPE[0], dr['rope'][0], 'rope', writes=[('rope', ph)])
            self.dma('pool', ROPE[1], dr['rope'][1], 'rope', writes=[('rope', ph)])

        self.mset('dve', qkT[:, 0:4, :], 0.0, writes=[('qz', ph)])
        for cc in range(4):
            for tt in range(4):
                tsl = slice(tt * 512, (tt + 1) * 512)
                akeys = K('a', range(NDC), range(tt * 4, tt * 4 + 4))
                bi = self.rr('pbG', 2)
                pa = self.pbank[bi]
                for dc in range(NDC):
                    self.mm(pa[:], W[:, dc, cc * 128:(cc + 1) * 128], self.aT[:, dc, tsl],
                            start=(dc == 0), stop=(dc == NDC - 1), reads=[('W', ph)] + akeys, writes=[('pb', bi)])
                if cc < 2:
                    dsts = [(slice(0, 64), 2 * cc), (slice(64, 128), 2 * cc + 1)]
                else:
                    dsts = [(slice(0, 128), 4 + cc - 2)]
                if is_m:
                    for n_, (psl, qi) in enumerate(dsts):
                        self.cp('act' if n_ == 0 else 'dve', qkT[psl, qi, tsl], pa[psl, :], reads=[('pb', bi), ('qz', ph)],
                                writes=K('qkT', ph, qi, range(tt * 4, tt * 4 + 4)))
                else:
                    pb2 = self.pbank[2 + bi]
                    for dc in range(NDC):
                        self.mm(pb2[:], W[:, dc, 1024 + cc * 128:1024 + (cc + 1) * 128], self.aT[:, dc, tsl],
                                start=(dc == 0), stop=(dc == NDC - 1), reads=[('Wsw', ph)] + akeys, writes=[('pb', 2 + bi)])
                    ri = self.rr('rtmp', 2)
                    self.tt('dve', rtmp[ri], pa[:], ROPE[0][:, tsl], ALU.mult, reads=[('pb', bi), ('rope', ph)],
                            writes=[('rtmp', ph, ri)])
                    self.tt('dve', rtmpB[ri], pb2[:], ROPE[1][:, tsl], ALU.mult, reads=[('pb', 2 + bi), ('rope', ph)],
                            writes=[('rtmpB', ph, ri)])
                    for (psl, qi) in dsts:
                        self.tt('pool', qkT[psl, qi, tsl], rtmp[ri][psl, :], rtmpB[ri][psl, :], ALU.add,
                                reads=[('rtmp', ph, ri), ('rtmpB', ph, ri), ('qz', ph)],
                                writes=K('qkT', ph, qi, range(tt * 4, tt * 4 + 4)))
        if is_m:
            self.mset('dve', vtok[:, :, :, 64:65], 1.0, writes=[('vone', ph)])
        for c in range(NCH):
            csl = slice(c * 128, (c + 1) * 128)
            akeys = K('a', range(NDC), c)
            bi = self.rr('pbG', 2)
            px = self.pbank[bi]
            py = self.pbank[2 + bi]
            for dc in range(NDC):
                self.mm(px[:], self.aT[:, dc, csl], W[:, dc, 512:1024], start=(dc == 0), stop=(dc == NDC - 1),
                        reads=[('W', ph)] + akeys, writes=[('pb', bi)])
            if is_m:
                for dc in range(NDC):
                    self.mm(py[:, 0:8], self.aT[:, dc, csl], W[:, dc, 1024:1032], start=(dc == 0), stop=(dc == NDC - 1),
                            reads=[('W', ph)] + akeys, writes=[('pb', 2 + bi)])
                self.tt('dve', G[:, c, :], py[:, 0:8], lp[:, 0:8], ALU.add, reads=[('pb', 2 + bi), ('lp', ph)],
                        writes=[('G', ph, c)])
            self.cp('act', vtok[:, c, :, 0:64], px[:, 0:256].rearrange("p (h e) -> p h e", h=4),
                    reads=[('pb', bi), ('vone', ph)] if is_m else [('pb', bi)], writes=[('vtok', ph, c)])
            self.act(gate[:, c, :], px[:, 256:512], AF.Sigmoid if is_m else AF.Silu, reads=[('pb', bi)],
                     writes=[('gate', ph, c)])
        STOP = os.environ.get('LIN_STOP', '')
        if STOP == 'B':
            return
        gk = [('G', ph, c) for c in range(NCH)]
        if is_m:
            self.act(TMP, G[:, :, 4:8], AF.Exp, scale=-1.0, reads=gk, writes=[('TMP', ph)])
            self.act(TMP, TMP, AF.Ln, bias=self.onec, reads=[('TMP', ph), ('cst',)], writes=[('TMP', ph)])
            self.ts('dve', LF, TMP, -1.0, ALU.mult, reads=[('TMP', ph)], writes=[('LF', ph)])
            self.gate_cumsum(LF, ('LF', ph), CG, ('CG', ph))
            self.act(QS, CG[:, :, 0:4], AF.Exp, reads=[('CG', ph)], writes=[('QS', ph)])
            self.act(DEC, CG[:, :, 4:8], AF.Exp, reads=[('CG', ph)], writes=[('DEC', ph)])
            self.tt('dve', TMP, CG[:, :, 4:8], CG[:, :, 0:4], ALU.subtract, reads=[('CG', ph), ('TMP', ph)], writes=[('TMP', ph)])
            self.tt('dve', TMP, TMP, G[:, :, 0:4], ALU.add, reads=[('TMP', ph)] + gk, writes=[('TMP', ph)])
            self.act(KS2, TMP, AF.Exp, reads=[('TMP', ph)], writes=[('KS2', ph)])
            self.ts('dve', KS2, KS2, 0.125, ALU.mult, reads=[('KS2', ph)], writes=[('KS2', ph)])
        else:
            self.cp('dve', QS, self.cst(C_RQS, 4).unsqueeze(1).to_broadcast([128, NCH, 4]), reads=[('cst',)], writes=[('QS', ph)])
            self.cp('dve', KS2, self.cst(C_RKS, 4).unsqueeze(1).to_broadcast([128, NCH, 4]), reads=[('cst',)], writes=[('KS2', ph)])
            self.cp('dve', DEC, self.cst(C_RDEC, 4).unsqueeze(1).to_broadcast([128, NCH, 4]), reads=[('cst',)], writes=[('DEC', ph)])
        if STOP == 'C':
            return
        self.mset('dve', C32, 0.0, writes=[('C32', ph)])
        self.mset('dve', Cb, 0.0, writes=[('Cb', ph)])
        gain = lp[:, 16 + m0:16 + m0 + 256].rearrange("p (h e) -> p h e", h=4)
        def lin_chunk(c, wi):
                csl = slice(c * 128, (c + 1) * 128)
                if is_m:
                    wT = wT_[wi]
                    bw = (4, 6)[self.rr('pbW', 2)]
                    pw = self.pbank[bw][:].rearrange("p (h i) -> p h i", h=4)
                    for h in range(4):
                        self.ts('dve', rt4_[wi][h], self.cst(C_TRIU), LF[:, c, h:h + 1], ALU.mult, reads=[('LF', ph), ('cst',)],
                                writes=[('rt4', ph, wi, h)])
                    yield
                    for h in range(4):
                        self.mm(pw[:, h, :], self.cst(C_MGT), rt4_[wi][h], start=True, stop=False,
                                reads=[('rt4', ph, wi, h), ('cst',)], writes=[('pb', bw)])
                        self.mm(pw[:, h, :], self.ident_f, self.cst(C_NEGT), start=False, stop=True, reads=[('cst',)], writes=[('pb', bw)])
                    for h in range(4):
                        self.act(wT[:, h, :], pw[:, h, :], AF.Exp, bias=G[:, c, h:h + 1], reads=[('pb', bw), ('G', ph, c)],
                                 writes=[('wT', ph, wi)])
                    wkey = [('wT', ph, wi)]
                else:
                    wT = self.cst(C_RW, 512).rearrange("p (h i) -> p h i", h=4)
                    wkey = [('cst',)]
                yield
                bs = self.rr('pbG', 2)
                psc = self.pbank[bs][:].rearrange("p (h i) -> p h i", h=4)
                for h in range(4):
                    self.mm(psc[:, h, :], qkT[:, 4 + h // 2, csl], qkT[:, h, csl],
                            reads=[('qkT', ph, 4 + h // 2, c), ('qkT', ph, h, c)], writes=[('pb', bs)])
                scm = scm_[wi]
                self.stt('dve', scm, psc, 0.125, wT, ALU.mult, ALU.mult, reads=[('pb', bs)] + wkey, writes=[('scm', ph, wi)])
                if STOP == 'D1':
                    return
                bk = 7
                pkt = self.pbank[bk][:].bitcast(BF16)[:, 0:256]
                for hh in range(2):
                    self.tr(pkt[:, hh * 128:(hh + 1) * 128], qkT[:, 4 + hh, csl], self.ident_b[:],
                            reads=[('qkT', ph, 4 + hh, c), ('ident_b',)], writes=[('pb', bk)])
                khat = khat_[wi]
                self.tt('dve', khat, pkt.rearrange("p (h e) -> p h e", h=4),
                        KS2[:, c, :].unsqueeze(2).to_broadcast([128, 4, 64]), ALU.mult,
                        reads=[('pb', bk), ('KS2', ph)], writes=[('khat', ph, wi)])
                if STOP == 'D2':
                    return
                yield
                bo = 2 + self.rr('pbO', 2)
                po1 = self.pbank[bo][:, 0:4 * E].rearrange("p (h e) -> p h e", h=4)
                bo2 = (4, 6)[self.rr('pbW', 2)]
                po2 = self.pbank[bo2][:, 0:4 * E].rearrange("p (h e) -> p h e", h=4)
                for h in range(4):
                    self.mm(po1[:, h, :], scm[:, h, :], vtok[:, c, h, :], reads=[('scm', ph, wi), ('vtok', ph, c)],
                            writes=[('pb', bo)])
                    self.mm(po2[:, h, :], qkT[:, h, csl], Cb[:, h // 2, :], reads=[('qkT', ph, h, c), ('Cb', ph)],
                            writes=[('pb', bo2)])
                T2, NUM = T2_[wi], NUM_[wi]
                self.tt('dve', T2, po2, QS[:, c, :].unsqueeze(2).to_broadcast([128, 4, E]), ALU.mult,
                        reads=[('pb', bo2), ('QS', ph)], writes=[('T2', ph, wi)])
                self.tt('dve', NUM, T2, po1, ALU.add, reads=[('T2', ph, wi), ('pb', bo)], writes=[('NUM', ph, wi)])
                if STOP == 'D3':
                    return
                if is_m:
                    R, HN = R_[wi], HN_[wi]
                    self.stt('dve', R, NUM[:, :, 64], -1.0, NUM[:, :, 64], ALU.mult, ALU.max, reads=[('NUM', ph, wi)], writes=[('R', ph, wi)])
                    self.ts('dve', R, R, 1.0, ALU.max, reads=[('R', ph, wi)], writes=[('R', ph, wi)])
                    self.rcp(R, R, reads=[('R', ph, wi)], writes=[('R', ph, wi)])
                    self.tt('dve', HN, NUM[:, :, 0:64], R.unsqueeze(2).to_broadcast([128, 4, 64]), ALU.mult,
                            reads=[('NUM', ph, wi), ('R', ph, wi)], writes=[('HN', ph, wi)])
                    hsrc, hkeys = HN, [('HN', ph, wi)]
                else:
                    hsrc, hkeys = NUM, [('NUM', ph, wi)]
                yield
                if STOP == 'D4':
                    return
                for hh in range(2):
                    bc = (4, 6)[self.rr('pbW', 2)]
                    pc = self.pbank[bc][:, 0:2 * E]
                    self.mm(pc, khat[:, 2 * hh:2 * hh + 2, :].rearrange("p h e -> p (h e)"),
                            vtok[:, c, 2 * hh:2 * hh + 2, :].rearrange("p h e -> p (h e)"),
                            reads=[('khat', ph, wi), ('vtok', ph, c)], writes=[('pb', bc)])
                    for q in range(2):
                        h = 2 * hh + q
                        hp = slice(q * 64, q * 64 + 64)
                        self.stt('dve', C32[hp, hh, :], C32[hp, hh, :], DEC[hp, c, h:h + 1], pc[hp, q * E:(q + 1) * E],
                                 ALU.mult, ALU.add, reads=[('C32', ph), ('DEC', ph), ('pb', bc)], writes=[('C32', ph)])
                self.cp('act', Cb, C32, reads=[('C32', ph)], writes=[('Cb', ph)])
                yield
                self.head_post(hsrc, hkeys, 4, 64, gain, gate[:, c, :].rearrange("p (h e) -> p h e", h=4), ('gate', ph, c),
                               ytok[:, c, :].rearrange("p (h e) -> p h e", h=4), ('gate', ph, c), post)

        active = []
        nxt = 0
        rounds = 0
        while nxt < NCH or active:
            if nxt < NCH and len(active) < KL and rounds % 2 == 0:
                active.append(lin_chunk(nxt, nxt % KL))
                nxt += 1
            for g_ in list(active):
                try:
                    next(g_)
                except StopIteration:
                    active.remove(g_)
            rounds += 1
        if STOP:
            return
        self.phase()
        self.view([128, LPW], F32)
        self.view([128, NDC, 1536], BF16)
        yT = self.view([128, 6, S], BF16)[:, 0:4, :]
        self.view([128, NCH, 4, E], BF16)
        ytok2 = self.view([128, NCH, 256], BF16)
        Wo = self.view([128, 4, D], BF16)
        for c in range(NCH):
            self.P.last_w[('ytok', self.ph, c)] = self.P.last_w.get(('gate', ph, c))
            self.P.last_r[('ytok', self.ph, c)] = {}
        self.emit_outproj(l, m0, 256, ytok2, yT, Wo)

    def emit_gdn(self, l):
        self.phase()
        ph = self.ph
        dr = self.dr
        Wd = dr['w_in'][l].rearrange("(dc p) f -> p dc f", p=128)
        lp = self.load_lp(l)
        ZS = self.view([128, NCH, 512], BF16)
        ytok = ZS
        Wz = self.view([128, NDC, 520], BF16)
        Wh = Wz[:, :, 0:384]
        ubase = self.apos
        RAW = self.view([128, 3, S + 4], BF16)
        ACC = self.view([128, S], F32)
        SQ = self.view([128, S], BF16)
        RN = [self.view([128, 512], F32) for _ in range(1)]
        uend1 = self.apos
        self.apos = ubase
        KI = 5
        TS = []
        for _ in range(KI):
            T = {}
            for nm in ('rt', 'dT', 'dS', 'N32', 'NbA', 'NbB', 'MbA', 'MbB', 'YA', 'YB', 'NUM', 'XT', 'A1'):
                T[nm] = self.view([128, 128], F32)
            T['CL'] = self.view([128, 3, 128], F32)
            for nm in ('rk', 'kh', 'bv', 'QKT', 'X', 'nWk', 'U'):
                T[nm] = self.view([128, 128], BF16)
            TS.append(T)
        SH = {nm: [self.view([128, 128], F32) for _ in range(2)] for nm in ('M32', 'T2')}
        self.apos = max(uend1, self.apos)
        CV = self.view([128, 3, S], BF16)
        GG = self.view([128, NCH, 8], F32)
        CG = self.view([128, NCH, 8], F32)
        LA = self.view([128, NCH, 4], F32)
        BETA = self.view([128, NCH, 4], F32)
        BEG = self.view([128, NCH, 4], F32)
        KH = self.view([128, NCH, 4], F32)
        DEC = self.view([128, NCH, 4], F32)
        QSG = self.view([128, NCH, 4], F32)
        TMP = self.view([128, NCH, 4], F32)
        NA = self.view([128, 4], F32)
        MSK = self.view([128, 4, 128], F32)
        S32 = self.view([128, 128], F32)
        Sb = self.view([128, 128], BF16)
        post = self.alloc_post(128)
        gcol = 2056
        self.dma('sp', MSK, dr['msk'].rearrange("p (a b) -> p a b", a=4), 'msk', writes=[('MSK', ph)])
        self.dma('pool', Wz[:, :, 0:520], Wd[:, :, 3592:4112], 'wz', writes=[('Wz', ph)])
        for c in range(NCH):
            csl = slice(c * 128, (c + 1) * 128)
            akeys = K('a', range(NDC), c)
            bi = self.rr('pbG', 2)
            px, py = self.pbank[bi], self.pbank[2 + bi]
            for dc in range(NDC):
                self.mm(px[:], self.aT[:, dc, csl], Wz[:, dc, 0:512], start=(dc == 0), stop=(dc == NDC - 1),
                        reads=[('Wz', ph)] + akeys, writes=[('pb', bi)])
            for dc in range(NDC):
                self.mm(py[:, 0:8], self.aT[:, dc, csl], Wz[:, dc, 512:520], start=(dc == 0), stop=(dc == NDC - 1),
                        reads=[('Wz', ph)] + akeys, writes=[('pb', 2 + bi)])
            self.act(ZS[:, c, :], px[:], AF.Silu, reads=[('pb', bi)], writes=[('ZS', ph, c)])
            self.cp('dve', GG[:, c, :], py[:, 0:8], reads=[('pb', 2 + bi)], writes=[('GG', ph, c)])
        ggk = [('GG', ph, c) for c in range(NCH)]
        self.tt('dve', TMP, GG[:, :, 0:4], lp[:, 12:16].unsqueeze(1).to_broadcast([128, NCH, 4]), ALU.add,
                reads=ggk + [('lp', ph)], writes=[('TMP', ph)])
        self.act(TMP, TMP, AF.Exp, reads=[('TMP', ph)], writes=[('TMP', ph)])
        self.act(TMP, TMP, AF.Ln, bias=self.onec, reads=[('TMP', ph), ('cst',)], writes=[('TMP', ph)])
        self.act(NA, lp[:, 8:12], AF.Exp, reads=[('lp', ph)], writes=[('NA', ph)])
        self.stt('dve', LA, TMP, -1.0, NA.unsqueeze(1).to_broadcast([128, NCH, 4]), ALU.mult, ALU.mult,
                 reads=[('TMP', ph), ('NA', ph)], writes=[('LA', ph)])
        self.act(BETA, GG[:, :, 4:8], AF.Sigmoid, reads=ggk, writes=[('BETA', ph)])
        self.gate_cumsum(LA, ('LA', ph), CG, ('CG', ph))
        self.act(QSG, CG[:, :, 0:4], AF.Exp, reads=[('CG', ph)], writes=[('QSG', ph)])
        self.tt('dve', BEG, QSG, BETA, ALU.mult, reads=[('QSG', ph), ('BETA', ph)], writes=[('BEG', ph)])
        self.ts('dve', QSG, QSG, 128.0 ** -0.5, ALU.mult, reads=[('QSG', ph), ('BEG', ph)], writes=[('QSG', ph)])
        self.act(DEC, CG[:, :, 4:8], AF.Exp, reads=[('CG', ph)], writes=[('DEC', ph)])
        self.tt('dve', TMP, CG[:, :, 4:8], CG[:, :, 0:4], ALU.subtract, reads=[('CG', ph), ('TMP', ph)], writes=[('TMP', ph)])
        self.act(KH, TMP, AF.Exp, reads=[('TMP', ph)], writes=[('KH', ph)])

        def load_wh(hh):
            for x3 in range(3):
                self.dma('pool', Wh[:, :, x3 * 128:(x3 + 1) * 128],
                         Wd[:, :, gcol + x3 * 512 + hh * 128:gcol + x3 * 512 + (hh + 1) * 128], 'wh', writes=[('Wz', ph)])

        for h in range(4):
            self.mset('dve', RAW[:, :, 0:3], 0.0, writes=[('rawpad', ph)])
            if h == 0:
                load_wh(0)
            for x3 in range(3):
                for tt in range(4):
                    tsl = slice(tt * 512, (tt + 1) * 512)
                    akeys = K('a', range(NDC), range(tt * 4, tt * 4 + 4))
                    bi = self.rr('pbG', 2)
                    pa = self.pbank[bi]
                    for dc in range(NDC):
                        self.mm(pa[:], Wh[:, dc, x3 * 128:(x3 + 1) * 128], self.aT[:, dc, tsl],
                                start=(dc == 0), stop=(dc == NDC - 1), reads=[('Wz', ph)] + akeys, writes=[('pb', bi)])
                    self.cp('act', RAW[:, x3, 3 + tt * 512:3 + (tt + 1) * 512], pa[:], reads=[('pb', bi), ('rawpad', ph)],
                            writes=[('RAW', ph, x3, tt)])
                if x3 == 2 and h < 3:
                    load_wh(h + 1)
                rawk = K('RAW', ph, x3, range(4))
                cw0 = 16 + 1024 + (x3 * 4 + h) * 4
                self.ts('dve', ACC, RAW[:, x3, 0:S], lp[:, cw0:cw0 + 1], ALU.mult, reads=rawk + [('lp', ph)], writes=[('ACC', ph)])
                for tap in range(1, 4):
                    self.stt('dve', ACC, RAW[:, x3, tap:tap + S], lp[:, cw0 + tap:cw0 + tap + 1], ACC, ALU.mult, ALU.add,
                             reads=rawk + [('lp', ph), ('ACC', ph)], writes=[('ACC', ph)])
                self.act(CV[:, x3, :], ACC, AF.Silu, reads=[('ACC', ph)], writes=K('CV', ph, x3, range(4)))
                if x3 < 2:
                    self.act(SQ, CV[:, x3, :], AF.Square, reads=K('CV', ph, x3, range(4)), writes=[('SQ', ph)])
                    for tt in range(4):
                        tsl = slice(tt * 512, (tt + 1) * 512)
                        pn = self.pbank[5]
                        self.mm(pn[:], self.ones_b[:], SQ[:, tsl], reads=[('SQ', ph), ('ones_b',)], writes=[('pb', 5)])
                        ri = 0
                        self.act(RN[ri], pn[:], AF.Sqrt, bias=self.epsc, reads=[('pb', 5), ('cst',)], writes=[('RN', ph, ri)])
                        self.rcp(RN[ri], RN[ri], reads=[('RN', ph, ri)], writes=[('RN', ph, ri)])
                        self.tt('dve', CV[:, x3, tsl], CV[:, x3, tsl], RN[ri], ALU.mult,
                                reads=[('CV', ph, x3, tt), ('RN', ph, ri)], writes=[('CV', ph, x3, tt)])
            self.mset('dve', S32, 0.0, writes=[('S32', ph)])
            self.mset('dve', Sb, 0.0, writes=[('Sb', ph)])
            gain = lp[:, 16 + 512 + h * 128:16 + 512 + (h + 1) * 128].unsqueeze(1)
            def chunk_gen(c, T, si):
                csl = slice(c * 128, (c + 1) * 128)
                tq = c // 4
                qn, kn, vt = CV[:, 0, csl], CV[:, 1, csl], CV[:, 2, csl]
                qk_keys = [('CV', ph, 0, tq), ('CV', ph, 1, tq)]
                kk = lambda nm: (nm, ph, si)
                bt = 7
                pt = self.pbank[bt][:].bitcast(BF16)[:, 0:256]
                self.tr(pt[:, 0:128], kn, self.ident_b[:], reads=[('CV', ph, 1, tq), ('ident_b',)], writes=[('pb', bt)])
                self.tr(pt[:, 128:256], vt, self.ident_b[:], reads=[('CV', ph, 2, tq), ('ident_b',)], writes=[('pb', bt)])
                self.ts('dve', T['rk'], pt[:, 0:128], BEG[:, c, h:h + 1], ALU.mult, reads=[('pb', bt), ('BEG', ph)], writes=[kk('rk')])
                self.ts('dve', T['kh'], pt[:, 0:128], KH[:, c, h:h + 1], ALU.mult, reads=[('pb', bt), ('KH', ph)], writes=[kk('kh')])
                self.ts('dve', T['bv'], pt[:, 128:256], BETA[:, c, h:h + 1], ALU.mult, reads=[('pb', bt), ('BETA', ph)], writes=[kk('bv')])
                r = T['rt']
                self.ts('dve', r, self.cst(C_TRIU), LA[:, c, h:h + 1], ALU.mult, reads=[('LA', ph), ('cst',)], writes=[kk('rt')])
                yield
                bT, bS = 4, 6
                pT, pS = self.pbank[bT][:, 0:128], self.pbank[bS][:, 0:128]
                self.mm(pT, self.cst(C_MGT), r, start=True, stop=False, reads=[kk('rt'), ('cst',)], writes=[('pb', bT)])
                self.mm(pT, self.ident_f, self.cst(C_NEGT), start=False, stop=True, reads=[('cst',)], writes=[('pb', bT)])
                self.mm(pS, r, self.cst(C_MGT), start=True, stop=False, reads=[kk('rt'), ('cst',)], writes=[('pb', bS)])
                self.mm(pS, self.ident_f, self.cst(C_NEGS), start=False, stop=True, reads=[('cst',)], writes=[('pb', bS)])
                self.act(T['dT'], pT, AF.Exp, reads=[('pb', bT)], writes=[kk('dT')])
                self.act(T['dS'], pS, AF.Exp, reads=[('pb', bS)], writes=[kk('dS')])
                yield
                bkk = self.rr('pbG', 2)
                pkk = self.pbank[bkk][:, 0:128]
                pqk = self.pbank[bkk][:, 128:256]
                self.mm(pkk, kn, kn, reads=[('CV', ph, 1, tq)], writes=[('pb', bkk)])
                self.mm(pqk, kn, qn, reads=qk_keys, writes=[('pb', bkk)])
                self.stt('dve', T['N32'], pkk, BETA[:, c, h:h + 1], T['dS'], ALU.mult, ALU.mult,
                         reads=[('pb', bkk), ('BETA', ph), kk('dS')], writes=[kk('N32')])
                self.stt('dve', T['QKT'], pqk, 128.0 ** -0.5, T['dT'], ALU.mult, ALU.mult, reads=[('pb', bkk), kk('dT')],
                         writes=[kk('QKT')])
                yield
                bm = 2 + self.rr('pbO', 2)
                pm = self.pbank[bm][:, 0:128]
                self.tr(pm, T['N32'], self.ident_f, reads=[kk('N32'), ('cst',)], writes=[('pb', bm)])
                m3i = self.rr('sh_m32', 2)
                M32, m32k = SH['M32'][m3i], ('M32', ph, m3i)
                self.cp('act', M32, pm, reads=[('pb', bm)], writes=[m32k])
                Nb, Mb, nbk, mbk = T['NbA'], T['MbA'], kk('NbA'), kk('MbA')
                Nb2, Mb2, nbk2, mbk2 = T['NbB'], T['MbB'], kk('NbB'), kk('MbB')
                self.tt('pool', Nb, T['N32'], MSK[:, 0, :], ALU.mult, reads=[kk('N32'), ('MSK', ph)], writes=[nbk])
                self.tt('pool', Mb, M32, MSK[:, 0, :], ALU.mult, reads=[m32k, ('MSK', ph)], writes=[mbk])
                self.tt('pool', T['CL'], T['N32'].unsqueeze(1).to_broadcast([128, 3, 128]), MSK[:, 1:4, :], ALU.mult,
                        reads=[kk('N32'), ('MSK', ph)], writes=[kk('CL')])
                Y, yk, Y2, yk2 = T['YA'], kk('YA'), T['YB'], kk('YB')
                self.tt('dve', Y, self.ident_f, Mb, ALU.subtract, reads=[mbk, ('cst',)], writes=[yk])
                yield
                for step in range(1, 4):
                    bn = self.rr('pbG', 2)
                    pn2 = self.pbank[bn][:, 0:128]
                    bm2 = 2 + self.rr('pbO', 2)
                    pm2 = self.pbank[bm2][:, 0:128]
                    self.mm(pn2, Mb, Nb, reads=[mbk, nbk], writes=[('pb', bn)])
                    if step < 3:
                        self.mm(pm2, Nb, Mb, reads=[mbk, nbk], writes=[('pb', bm2)])
                    self.cp('act', Nb2, pn2, reads=[('pb', bn)], writes=[nbk2])
                    if step < 3:
                        self.cp('dve', Mb2, pm2, reads=[('pb', bm2)], writes=[mbk2])
                        Mb, Mb2, mbk, mbk2 = Mb2, Mb, mbk2, mbk
                    Nb, Nb2, nbk, nbk2 = Nb2, Nb, nbk2, nbk
                    yield
                    bx = (4, 6)[self.rr('pbW', 2)]
                    px2 = self.pbank[bx][:, 0:128]
                    self.mm(px2, Nb, Y, reads=[nbk, yk], writes=[('pb', bx)])
                    self.tt('dve', Y2, px2, Y, ALU.add, reads=[('pb', bx), yk], writes=[yk2])
                    Y, Y2, yk, yk2 = Y2, Y, yk2, yk
                    yield
                for lvl in range(3):
                    bt2 = 2 + self.rr('pbO', 2)
                    pxt = self.pbank[bt2][:, 0:128]
                    ba1 = self.rr('pbG', 2)
                    pa1 = self.pbank[ba1][:, 0:128]
                    self.tr(pxt, Y, self.ident_f, reads=[yk, ('cst',)], writes=[('pb', bt2)])
                    self.mm(pa1, T['CL'][:, lvl, :], Y, reads=[kk('CL'), yk], writes=[('pb', ba1)])
                    XT, A1, xtk, a1k = T['XT'], T['A1'], kk('XT'), kk('A1')
                    self.cp('act', XT, pxt, reads=[('pb', bt2)], writes=[xtk])
                    self.cp('dve', A1, pa1, reads=[('pb', ba1)], writes=[a1k])
                    yield
                    bx = (4, 6)[self.rr('pbW', 2)]
                    px2 = self.pbank[bx][:, 0:128]
                    self.mm(px2, XT, A1, reads=[xtk, a1k], writes=[('pb', bx)])
                    self.tt('dve', Y2, Y, px2, ALU.subtract, reads=[('pb', bx), yk], writes=[yk2])
                    Y, Y2, yk, yk2 = Y2, Y, yk2, yk
                    yield
                X = T['X']
                self.cp('act', X, Y, reads=[yk], writes=[kk('X')])
                yield
                bwk = self.rr('pbG', 2)
                pwk = self.pbank[bwk][:, 0:128]
                self.mm(pwk, T['rk'], X, reads=[kk('rk'), kk('X')], writes=[('pb', bwk)])
                self.act(T['nWk'], pwk, AF.Copy, scale=-1.0, reads=[('pb', bwk)], writes=[kk('nWk')])
                yield
                bu = 2 + self.rr('pbO', 2)
                pu = self.pbank[bu][:, 0:128]
                self.mm(pu, X, T['bv'], start=True, stop=False, reads=[kk('X'), kk('bv')], writes=[('pb', bu)])
                self.mm(pu, T['nWk'], Sb, start=False, stop=True, reads=[kk('nWk'), ('Sb', ph)], writes=[('pb', bu)])
                U = T['U']
                self.cp('act', U, pu, reads=[('pb', bu)], writes=[kk('U')])
                yield
                bo1 = self.rr('pbG', 2)
                po1 = self.pbank[bo1][:, 0:128]
                po2 = self.pbank[bo1][:, 128:256]
                pss = self.pbank[bo1][:, 256:384]
                self.mm(po2, qn, Sb, reads=[('CV', ph, 0, tq), ('Sb', ph)], writes=[('pb', bo1)])
                self.mm(pss, T['kh'], U, reads=[kk('kh'), kk('U')], writes=[('pb', bo1)])
                self.mm(po1, T['QKT'], U, reads=[kk('QKT'), kk('U')], writes=[('pb', bo1)])
                self.stt('dve', S32, S32, DEC[:, c, h:h + 1], pss, ALU.mult, ALU.add,
                         reads=[('S32', ph), ('DEC', ph), ('pb', bo1)], writes=[('S32', ph)])
                self.cp('act', Sb, S32, reads=[('S32', ph)], writes=[('Sb', ph)])
                t2i = self.rr('sh_t2', 2)
                T2, t2k = SH['T2'][t2i], ('T2', ph, t2i)
                self.ts('dve', T2, po2, QSG[:, c, h:h + 1], ALU.mult, reads=[('pb', bo1), ('QSG', ph)], writes=[t2k])
                self.tt('dve', T['NUM'], T2, po1, ALU.add, reads=[t2k, ('pb', bo1)], writes=[kk('NUM')])
                yield
                self.head_post(T['NUM'].unsqueeze(1), [kk('NUM')], 1, 128, gain,
                               ZS[:, c, h * 128:(h + 1) * 128].unsqueeze(1), ('ZS', ph, c),
                               ytok[:, c, h * 128:(h + 1) * 128].unsqueeze(1), ('ZS', ph, c), post)

            self.P.barrier()
            active = []
            nxt = 0
            rounds = 0
            START_EVERY = 3
            while nxt < NCH or active:
                if nxt < NCH and len(active) < KI and rounds % START_EVERY == 0:
                    active.append(chunk_gen(nxt, TS[nxt % KI], nxt % KI))
                    nxt += 1
                for g_ in list(active):
                    try:
                        next(g_)
                    except StopIteration:
                        active.remove(g_)
                rounds += 1
            self.P.barrier()
        self.phase()
        self.view([128, LPW], F32)
        ytok2 = self.view([128, NCH, 512], BF16)
        yT = self.view([128, 4, S], BF16)
        Wo = self.view([128, 4, D], BF16)
        for c in range(NCH):
            self.P.last_w[('ytok', self.ph, c)] = self.P.last_w.get(('ZS', ph, c))
            self.P.last_r[('ytok', self.ph, c)] = {}
        self.emit_outproj(l, 512, 512, ytok2, yT, Wo)


def host_consts(inputs, nlayers):
    f32 = np.float32
    g = np.stack([inputs['ffn1_norm'], inputs['mix_norm'], inputs['ffn2_norm']], axis=1).reshape(3 * NL, D)
    g = np.concatenate([g, inputs['final_norm'][None, :]], axis=0)
    gains = np.ascontiguousarray(g.reshape(3 * NL + 1, NDC, 128).transpose(2, 0, 1)).astype(f32)
    idx = np.arange(128)
    cst = np.zeros((128, CW), f32)
    cst[:, C_ID:C_ID + 128] = np.eye(128)
    cst[:, C_TRIU:C_TRIU + 128] = (idx[:, None] <= idx[None, :])
    cst[:, C_MGT:C_MGT + 128] = (idx[:, None] > idx[None, :])
    cst[:, C_NEGT:C_NEGT + 128] = np.where(idx[None, :] < idx[:, None], NEG, 0.0)
    cst[:, C_NEGS:C_NEGS + 128] = np.where(idx[None, :] >= idx[:, None], NEG, 0.0)
    cst[:, C_ONES:C_ONES + 128] = 1.0
    lg = np.log1p(-np.exp2(-5.0 - np.arange(4, dtype=np.float64)))
    rel = (idx[None, :] - idx[:, None]).astype(np.float64)
    for h in range(4):
        w = np.where(rel >= 0, np.exp(np.where(rel >= 0, rel, 0.0) * lg[h]), 0.0)
        cst[:, C_RW + h * 128:C_RW + (h + 1) * 128] = w
        cst[:, C_RQS + h] = np.exp((idx + 1.0) * lg[h])
        cst[:, C_RKS + h] = np.exp((127.0 - idx) * lg[h]) * 0.125
        cst[:, C_RDEC + h] = np.exp(128.0 * lg[h])
    cst[:, C_EPS] = EPS
    inv_freq = (10000.0 ** (-np.arange(0, 64, 2, dtype=f32) / f32(64))).astype(f32)
    ang = (np.arange(S, dtype=f32)[None, :] * inv_freq[:, None]).astype(f32).astype(np.float64)
    cosr = np.cos(ang)
    sinr = np.sin(ang)
    rope = np.zeros((2, 128, S), f32)
    for p in range(128):
        f = p % 32
        rope[0, p] = cosr[f]
        rope[1, p] = (-sinr[f] if (p % 64) < 32 else sinr[f])
    lp = np.zeros((nlayers, 128, LPW), f32)
    for l in range(nlayers):
        lp[l, :, 0:4] = inputs['m_i_bias'][l][None, :]
        lp[l, :, 4:8] = inputs['m_f_bias'][l][None, :]
        lp[l, :, 8:12] = inputs['g_a_log'][l][None, :]
        lp[l, :, 12:16] = inputs['g_dt_bias'][l][None, :]
        lp[l, :, 16:16 + 256] = inputs['m_out_norm'][l][None, :]
        lp[l, :, 16 + 256:16 + 512] = inputs['r_out_norm'][l][None, :]
        lp[l, :, 16 + 512:16 + 1024] = inputs['g_out_norm'][l][None, :]
        cw = inputs['g_conv'][l]
        for x3 in range(3):
            for h in range(4):
                ch = x3 * 512 + h * 128 + idx
                lp[l, :, 16 + 1024 + (x3 * 4 + h) * 4:16 + 1024 + (x3 * 4 + h) * 4 + 4] = cw[:, ch].T
    msk = np.zeros((128, 4, 128), f32)
    blk = lambda b: (idx[:, None] // b == idx[None, :] // b).astype(f32)
    msk[:, 0] = blk(16)
    msk[:, 1] = blk(32) - blk(16)
    msk[:, 2] = blk(64) - blk(32)
    msk[:, 3] = blk(128) - blk(64)
    return {'gains': gains, 'cst': cst, 'rope': rope, 'lp': lp, 'msk': msk.reshape(128, 512)}


_CACHE = {}


def run(inputs, nseq_per_core=4, nlayers=NL, stages=('ffn1', 'mix', 'ffn2'), ncores=8, trace=False):
    key = (nseq_per_core, nlayers, tuple(stages))
    if key not in _CACHE:
        _CACHE[key] = Builder(nseq_per_core, nlayers, stages).build()
    nc = _CACHE[key]
    consts = host_consts(inputs, nlayers)
    x = np.ascontiguousarray(inputs['x'], dtype=np.float32)
    in_maps = []
    shared = {}
    for f in (1, 2):
        for w in ('w_gate', 'w_up', 'w_down'):
            shared[f'ffn{f}_{w}'] = np.ascontiguousarray(inputs[f'ffn{f}_{w}'][:nlayers], dtype=np.float32)
    shared['w_in'] = np.ascontiguousarray(inputs['w_in'][:nlayers], dtype=np.float32)
    shared['w_out'] = np.ascontiguousarray(inputs['w_out'][:nlayers], dtype=np.float32)
    for c in range(ncores):
        m = {'x': x[c * nseq_per_core:(c + 1) * nseq_per_core]}
        m.update(shared)
        m.update(consts)
        in_maps.append(m)
    res = run_bass_kernel_spmd(nc, in_maps, core_ids=list(range(ncores)), trace=trace)
    out = np.concatenate([r['out'] for r in res.results], axis=0)
    return out, res


def kernel(**inputs):
    out, _ = run(inputs)
    return out.astype(np.float32)
```
